# Optimizing a Trainium2 kernel written in Bass

```python
import math, functools
import jax, jax.numpy as jnp
from jax import lax
import numpy as np

D_MODEL = 1024
BATCH = 8
SEQ = 4096
DEPTH = 4

GRID_W = 64
CTX_LEN = 256
N_MOD = 6
RMS_EPS = 1e-6
N_HEADS = 16
N_KV_HEADS = 4
HEAD_DIM = D_MODEL // N_HEADS
GROUP = N_HEADS // N_KV_HEADS
Q_DIM = N_HEADS * HEAD_DIM
KV_DIM = N_KV_HEADS * HEAD_DIM
ROPE_AXIS_DIM = HEAD_DIM // 2
ROPE_THETA = 10000.0
Q_BLOCK = 128
SSM_INNER = 2 * D_MODEL
SSM_HEAD_DIM = 64
SSM_HEADS = SSM_INNER // SSM_HEAD_DIM
SSM_GROUPS = 4
SSM_STATE = 128
SSM_CONV_WIDTH = 5
SSM_CONV_DIM = SSM_INNER + 2 * SSM_GROUPS * SSM_STATE
SSM_IN_DIM = SSM_INNER + SSM_CONV_DIM + 2 * SSM_HEADS
SSM_CHUNK = 128
SC_WIDTH = 3
D_FF = 3584
N_EXPERTS = 8
TOP_K = 2

kernel_name = 'hybrid_attn_ssd_shortconv_moe_dit'


def rms_norm(x, gain):
    x32 = x.astype(jnp.float32)
    y = x32 * lax.rsqrt(jnp.mean(x32 * x32, axis=-1, keepdims=True) + RMS_EPS)
    return (y * gain.astype(jnp.float32)).astype(x.dtype)


def modulate(h, shift, scale):
    return h * (1.0 + scale) + shift


def depthwise_conv(x, w):
    width = w.shape[0]
    return lax.conv_general_dilated(
        x, w[:, None, :].astype(x.dtype), (1,), [(width // 2, width // 2)],
        dimension_numbers=('NWC', 'WIO', 'NWC'), feature_group_count=x.shape[-1])


def axial_rope_tables(n_tok):
    rows = n_tok // GRID_W
    row = jnp.repeat(jnp.arange(rows), GRID_W).astype(jnp.float32)
    col = jnp.tile(jnp.arange(GRID_W), rows).astype(jnp.float32)
    inv = 1.0 / (ROPE_THETA ** (jnp.arange(0, ROPE_AXIS_DIM, 2, dtype=jnp.float32) / ROPE_AXIS_DIM))
    ang = jnp.stack([row[:, None] * inv, col[:, None] * inv], axis=1)
    ang = jnp.broadcast_to(ang[:, :, None, :], (n_tok, 2, 2, ROPE_AXIS_DIM // 2)).reshape(n_tok, HEAD_DIM)
    return jnp.cos(ang), jnp.sin(ang)


def apply_rope(x, cos, sin):
    xs = x.reshape(*x.shape[:-1], 2, 2, ROPE_AXIS_DIM // 2)
    rot = jnp.concatenate([-xs[..., 1:, :], xs[..., :1, :]], axis=-2).reshape(x.shape)
    return x * cos[:, None, :].astype(x.dtype) + rot * sin[:, None, :].astype(x.dtype)


def gqa_attend(q, k, v):
    s = jnp.einsum('bqkgd,bskd->bkgqs', q, k).astype(jnp.float32) * (HEAD_DIM ** -0.5)
    p = jax.nn.softmax(s, axis=-1).astype(v.dtype)
    return jnp.einsum('bkgqs,bskd->bqkgd', p, v)


def attention_mixer(h_lat, h_ctx, need_ctx, wqkv, q_norm, k_norm, wo):
    bsz, n_lat, _ = h_lat.shape
    n_ctx = h_ctx.shape[1]

    def project(h):
        q, k, v = jnp.split(h @ wqkv, [Q_DIM, Q_DIM + KV_DIM], axis=-1)
        bl = h.shape[:2]
        q = rms_norm(q.reshape(*bl, N_HEADS, HEAD_DIM), q_norm)
        k = rms_norm(k.reshape(*bl, N_KV_HEADS, HEAD_DIM), k_norm)
        return q, k, v.reshape(*bl, N_KV_HEADS, HEAD_DIM)

    q_l, k_l, v_l = project(h_lat)
    q_c, k_c, v_c = project(h_ctx)
    cos, sin = axial_rope_tables(n_lat)
    q_l = apply_rope(q_l, cos, sin)
    k_l = apply_rope(k_l, cos, sin)
    k_all = jnp.concatenate([k_c, k_l], axis=1)
    v_all = jnp.concatenate([v_c, v_l], axis=1)
    n_blk = n_lat // Q_BLOCK
    q_blk = q_l.reshape(bsz, n_blk, Q_BLOCK, N_KV_HEADS, GROUP, HEAD_DIM).transpose(1, 0, 2, 3, 4, 5)
    o_blk = lax.map(lambda qb: gqa_attend(qb, k_all, v_all), q_blk)
    o_lat = o_blk.transpose(1, 0, 2, 3, 4, 5).reshape(bsz, n_lat, Q_DIM) @ wo
    o_ctx = None
    if need_ctx:
        o_c = gqa_attend(q_c.reshape(bsz, n_ctx, N_KV_HEADS, GROUP, HEAD_DIM), k_c, v_c)
        o_ctx = o_c.reshape(bsz, n_ctx, Q_DIM) @ wo
    return o_lat, o_ctx


def ssd_scan(x, dt, a, b_in, c_in, h0):
    f32 = jnp.float32
    bsz, n_tok = x.shape[:2]
    nc = n_tok // SSM_CHUNK
    r = SSM_HEADS // SSM_GROUPS
    dt = dt.astype(f32)
    xdt = (x.astype(f32) * dt[..., None]).reshape(bsz, nc, SSM_CHUNK, SSM_GROUPS, r, SSM_HEAD_DIM)
    adt = (dt * a.astype(f32)).reshape(bsz, nc, SSM_CHUNK, SSM_GROUPS, r)
    acs = jnp.cumsum(jnp.moveaxis(adt, 2, -1), axis=-1)
    bc = b_in.astype(f32).reshape(bsz, nc, SSM_CHUNK, SSM_GROUPS, SSM_STATE)
    cc = c_in.astype(f32).reshape(bsz, nc, SSM_CHUNK, SSM_GROUPS, SSM_STATE)
    lower = jnp.tril(jnp.ones((SSM_CHUNK, SSM_CHUNK), dtype=bool))
    decay = jnp.exp(jnp.where(lower, acs[..., :, None] - acs[..., None, :], -jnp.inf))
    cb = jnp.einsum('bclgn,bcsgn->bcgls', cc, bc)
    y_diag = jnp.einsum('bcgls,bcgrls,bcsgrp->bclgrp', cb, decay, xdt)
    decay_to_end = jnp.exp(acs[..., -1:] - acs)
    states = jnp.einsum('bclgn,bcgrl,bclgrp->bcgrpn', bc, decay_to_end, xdt)
    chunk_decay = jnp.exp(acs[..., -1])

    def step(h, inp):
        st, dec = inp
        return h * dec[..., None, None] + st, h

    h_last, h_prev = lax.scan(
        step, h0.astype(f32).reshape(bsz, SSM_GROUPS, r, SSM_HEAD_DIM, SSM_STATE),
        (jnp.moveaxis(states, 1, 0), jnp.moveaxis(chunk_decay, 1, 0)))
    h_prev = jnp.moveaxis(h_prev, 0, 1)
    y_off = jnp.einsum('bclgn,bcgrpn,bcgrl->bclgrp', cc, h_prev, jnp.exp(acs))
    y = (y_diag + y_off).reshape(bsz, n_tok, SSM_HEADS, SSM_HEAD_DIM).astype(x.dtype)
    return y, h_last.reshape(bsz, SSM_HEADS, SSM_HEAD_DIM, SSM_STATE)


def ssd_mixer(h_lat, h_ctx, need_ctx, in_proj, conv_w, conv_b, a_log_fwd, a_log_bwd,
              dt_bias_fwd, dt_bias_bwd, d_skip, out_norm, out_proj):
    def prep(h):
        z, xbc, dt_f, dt_b = jnp.split(
            h @ in_proj, [SSM_INNER, SSM_INNER + SSM_CONV_DIM, SSM_INNER + SSM_CONV_DIM + SSM_HEADS], axis=-1)
        xbc = jax.nn.silu(depthwise_conv(xbc, conv_w) + conv_b)
        xs, bs, cs = jnp.split(xbc, [SSM_INNER, SSM_INNER + SSM_GROUPS * SSM_STATE], axis=-1)
        bl = h.shape[:2]
        dt_f = jax.nn.softplus((dt_f + dt_bias_fwd).astype(jnp.float32))
        dt_b = jax.nn.softplus((dt_b + dt_bias_bwd).astype(jnp.float32))
        return (z, xs.reshape(*bl, SSM_HEADS, SSM_HEAD_DIM), bs.reshape(*bl, SSM_GROUPS, SSM_STATE),
                cs.reshape(*bl, SSM_GROUPS, SSM_STATE), dt_f, dt_b)

    z_c, x_c, b_c, c_c, dtf_c, dtb_c = prep(h_ctx)
    z_l, x_l, b_l, c_l, dtf_l, dtb_l = prep(h_lat)
    a_f = -jnp.exp(a_log_fwd.astype(jnp.float32))
    a_b = -jnp.exp(a_log_bwd.astype(jnp.float32))
    h0 = jnp.zeros((h_lat.shape[0], SSM_HEADS, SSM_HEAD_DIM, SSM_STATE), jnp.float32)
    flip = lambda t: jnp.flip(t, axis=1)
    yf_c, s_f = ssd_scan(x_c, dtf_c, a_f, b_c, c_c, h0)
    yf_l, _ = ssd_scan(x_l, dtf_l, a_f, b_l, c_l, s_f)
    yb_c, s_b = ssd_scan(flip(x_c), flip(dtb_c), a_b, flip(b_c), flip(c_c), h0)
    yb_l, _ = ssd_scan(flip(x_l), flip(dtb_l), a_b, flip(b_l), flip(c_l), s_b)

    def finish(y_f, y_b, xs, z):
        y = y_f + y_b + xs * d_skip[:, None]
        y = y.reshape(*z.shape[:2], SSM_INNER)
        return rms_norm(y * jax.nn.silu(z), out_norm) @ out_proj

    o_lat = finish(yf_l, flip(yb_l), x_l, z_l)
    o_ctx = finish(yf_c, flip(yb_c), x_c, z_c) if need_ctx else None
    return o_lat, o_ctx


def short_conv_mixer(h_lat, h_ctx, need_ctx, in_proj, conv_w, out_proj):
    def mix(h):
        b_gate, c_gate, xt = jnp.split(h @ in_proj, 3, axis=-1)
        return (b_gate * depthwise_conv(c_gate * xt, conv_w)) @ out_proj
    return mix(h_lat), (mix(h_ctx) if need_ctx else None)


def swiglu(h, w_gu, w_down):
    g, u = jnp.split(h @ w_gu, 2, axis=-1)
    return (jax.nn.silu(g) * u) @ w_down


def moe_swiglu(h, router, router_b, w_gu, w_down):
    logits = (h @ router + router_b).astype(jnp.float32)
    top_v, top_i = lax.top_k(logits, TOP_K)
    top_w = jax.nn.softmax(top_v, axis=-1)
    gates = jnp.sum(jax.nn.one_hot(top_i, N_EXPERTS, dtype=jnp.float32) * top_w[..., None], axis=-2).astype(h.dtype)
    out = jnp.zeros_like(h)
    for e in range(N_EXPERTS):
        out = out + gates[..., e:e + 1] * swiglu(h, w_gu[e], w_down[e])
    return out


def setup_inputs(seed: int = 0) -> dict:
    key = jax.random.key(seed)
    ks = iter(jax.random.split(key, 128))

    def nrm(shape, scale):
        return jax.random.normal(next(ks), shape, jnp.float32) * scale

    def gain(n, s=0.02):
        return 1.0 + s * jax.random.normal(next(ks), (n,), jnp.float32)

    def dt_bias(n):
        dt = jnp.exp(jax.random.uniform(next(ks), (n,), jnp.float32, math.log(1e-3), math.log(1e-1)))
        return dt + jnp.log(-jnp.expm1(-dt))

    def a_log(n):
        return jnp.log(jax.random.uniform(next(ks), (n,), jnp.float32, 1.0, 16.0))

    inp = {}
    inp['x'] = nrm((BATCH, SEQ, D_MODEL), 1.0)
    inp['c'] = nrm((BATCH, D_MODEL), 1.0)
    inp['ctx'] = nrm((BATCH, CTX_LEN, D_MODEL), 1.0)
    inp['c_ctx'] = nrm((D_MODEL,), 1.0)
    dsc = D_MODEL ** -0.5
    for i in range(DEPTH):
        p = 'l%d_' % i
        inp[p + 'ada_w'] = nrm((D_MODEL, N_MOD * D_MODEL), 0.5 * dsc)
        inp[p + 'ada_b'] = nrm((N_MOD * D_MODEL,), 0.01)
        inp[p + 'norm_mix'] = gain(D_MODEL)
        inp[p + 'norm_ffn'] = gain(D_MODEL)
        kind = i % 3
        if kind == 0:
            inp[p + 'wqkv'] = nrm((D_MODEL, Q_DIM + 2 * KV_DIM), dsc)
            inp[p + 'q_norm'] = gain(HEAD_DIM)
            inp[p + 'k_norm'] = gain(HEAD_DIM)
            inp[p + 'wo'] = nrm((Q_DIM, D_MODEL), Q_DIM ** -0.5)
        elif kind == 1:
            inp[p + 'ssm_in_proj'] = nrm((D_MODEL, SSM_IN_DIM), dsc)
            inp[p + 'ssm_conv_w'] = nrm((SSM_CONV_WIDTH, SSM_CONV_DIM), SSM_CONV_WIDTH ** -0.5)
            inp[p + 'ssm_conv_b'] = nrm((SSM_CONV_DIM,), 0.01)
            inp[p + 'ssm_a_log_fwd'] = a_log(SSM_HEADS)
            inp[p + 'ssm_a_log_bwd'] = a_log(SSM_HEADS)
            inp[p + 'ssm_dt_bias_fwd'] = dt_bias(SSM_HEADS)
            inp[p + 'ssm_dt_bias_bwd'] = dt_bias(SSM_HEADS)
            inp[p + 'ssm_d_skip'] = gain(SSM_HEADS, 0.1)
            inp[p + 'ssm_out_norm'] = gain(SSM_INNER)
            inp[p + 'ssm_out_proj'] = nrm((SSM_INNER, D_MODEL), SSM_INNER ** -0.5)
        else:
            inp[p + 'sc_in_proj'] = nrm((D_MODEL, 3 * D_MODEL), dsc)
            inp[p + 'sc_conv_w'] = nrm((SC_WIDTH, D_MODEL), SC_WIDTH ** -0.5)
            inp[p + 'sc_out_proj'] = nrm((D_MODEL, D_MODEL), dsc)
        if i % 2 == 0:
            inp[p + 'ffn_w_gu'] = nrm((D_MODEL, 2 * D_FF), dsc)
            inp[p + 'ffn_w_down'] = nrm((D_FF, D_MODEL), D_FF ** -0.5)
        else:
            inp[p + 'moe_router'] = nrm((D_MODEL, N_EXPERTS), dsc)
            inp[p + 'moe_router_b'] = nrm((N_EXPERTS,), 0.01)
            inp[p + 'moe_w_gu'] = nrm((N_EXPERTS, D_MODEL, 2 * D_FF), dsc)
            inp[p + 'moe_w_down'] = nrm((N_EXPERTS, D_FF, D_MODEL), D_FF ** -0.5)
    return inp


def reference(x, c, ctx, c_ctx,
              l0_ada_w, l0_ada_b, l0_norm_mix, l0_norm_ffn, l0_wqkv, l0_q_norm, l0_k_norm, l0_wo,
              l0_ffn_w_gu, l0_ffn_w_down,
              l1_ada_w, l1_ada_b, l1_norm_mix, l1_norm_ffn, l1_ssm_in_proj, l1_ssm_conv_w, l1_ssm_conv_b,
              l1_ssm_a_log_fwd, l1_ssm_a_log_bwd, l1_ssm_dt_bias_fwd, l1_ssm_dt_bias_bwd, l1_ssm_d_skip,
              l1_ssm_out_norm, l1_ssm_out_proj, l1_moe_router, l1_moe_router_b, l1_moe_w_gu, l1_moe_w_down,
              l2_ada_w, l2_ada_b, l2_norm_mix, l2_norm_ffn, l2_sc_in_proj, l2_sc_conv_w, l2_sc_out_proj,
              l2_ffn_w_gu, l2_ffn_w_down,
              l3_ada_w, l3_ada_b, l3_norm_mix, l3_norm_ffn, l3_wqkv, l3_q_norm, l3_k_norm, l3_wo,
              l3_moe_router, l3_moe_router_b, l3_moe_w_gu, l3_moe_w_down):
    mixers = [
        functools.partial(attention_mixer, wqkv=l0_wqkv, q_norm=l0_q_norm, k_norm=l0_k_norm, wo=l0_wo),
        functools.partial(ssd_mixer, in_proj=l1_ssm_in_proj, conv_w=l1_ssm_conv_w, conv_b=l1_ssm_conv_b,
                          a_log_fwd=l1_ssm_a_log_fwd, a_log_bwd=l1_ssm_a_log_bwd,
                          dt_bias_fwd=l1_ssm_dt_bias_fwd, dt_bias_bwd=l1_ssm_dt_bias_bwd,
                          d_skip=l1_ssm_d_skip, out_norm=l1_ssm_out_norm, out_proj=l1_ssm_out_proj),
        functools.partial(short_conv_mixer, in_proj=l2_sc_in_proj, conv_w=l2_sc_conv_w, out_proj=l2_sc_out_proj),
        functools.partial(attention_mixer, wqkv=l3_wqkv, q_norm=l3_q_norm, k_norm=l3_k_norm, wo=l3_wo),
    ]
    channels = [
        functools.partial(swiglu, w_gu=l0_ffn_w_gu, w_down=l0_ffn_w_down),
        functools.partial(moe_swiglu, router=l1_moe_router, router_b=l1_moe_router_b,
                          w_gu=l1_moe_w_gu, w_down=l1_moe_w_down),
        functools.partial(swiglu, w_gu=l2_ffn_w_gu, w_down=l2_ffn_w_down),
        functools.partial(moe_swiglu, router=l3_moe_router, router_b=l3_moe_router_b,
                          w_gu=l3_moe_w_gu, w_down=l3_moe_w_down),
    ]
    adaln = [(l0_ada_w, l0_ada_b, l0_norm_mix, l0_norm_ffn), (l1_ada_w, l1_ada_b, l1_norm_mix, l1_norm_ffn),
             (l2_ada_w, l2_ada_b, l2_norm_mix, l2_norm_ffn), (l3_ada_w, l3_ada_b, l3_norm_mix, l3_norm_ffn)]
    x_lat, x_ctx = x, ctx
    for i in range(DEPTH):
        ada_w, ada_b, g_mix, g_ffn = adaln[i]
        need_ctx = i < DEPTH - 1
        sh1_l, sc1_l, gt1_l, sh2_l, sc2_l, gt2_l = jnp.split(
            (jax.nn.silu(c) @ ada_w + ada_b)[:, None, :], N_MOD, axis=-1)
        sh1_c, sc1_c, gt1_c, sh2_c, sc2_c, gt2_c = jnp.split(
            (jax.nn.silu(c_ctx) @ ada_w + ada_b)[None, None, :], N_MOD, axis=-1)
        y_lat, y_ctx = mixers[i](modulate(rms_norm(x_lat, g_mix), sh1_l, sc1_l),
                                 modulate(rms_norm(x_ctx, g_mix), sh1_c, sc1_c), need_ctx)
        x_lat = x_lat + gt1_l * y_lat
        x_lat = x_lat + gt2_l * channels[i](modulate(rms_norm(x_lat, g_ffn), sh2_l, sc2_l))
        if need_ctx:
            x_ctx = x_ctx + gt1_c * y_ctx
            x_ctx = x_ctx + gt2_c * channels[i](modulate(rms_norm(x_ctx, g_ffn), sh2_c, sc2_c))
    return x_lat
```

```python
import contextlib
import numpy as np
import concourse.bass as bass
import concourse.mybir as mybir
from concourse.bass_utils import run_bass_kernel_spmd

F32 = mybir.dt.float32
BF16 = mybir.dt.bfloat16
AF = mybir.ActivationFunctionType
ALU = mybir.AluOpType
AX = mybir.AxisListType

D = 1024
NB = 8
SEQ = 4096
CTX = 256
T = SEQ + CTX
DFF = 3584
NE = 8
NH = 16
NKV = 4
DH = 64
EPS = 1e-6


class Res:
    __slots__ = ("w", "r")

    def __init__(self):
        self.w = None
        self.r = {}


class Eng:
    def __init__(self, nc, es, eng, name, self_raw):
        self.eng = eng
        self.name = name
        self.sem = es.enter_context(nc.semaphore("sem_" + name))
        self.cnt = 0
        self.seen = {}
        self.self_raw = self_raw
        self.pending = False


class DSem:
    def __init__(self, nc, es, name):
        self.sem = es.enter_context(nc.semaphore("dsem_" + name))
        self.cnt = 0
        self.name = name


def _add(need, mark):
    src, val = mark
    if need.get(src, 0) < val:
        need[src] = val


class K:
    def __init__(self):
        self.nc = bass.Bass("TRN2", target_bir_lowering=False)
        nc = self.nc
        self.es = contextlib.ExitStack()
        es = self.es
        self.PE = Eng(nc, es, nc.tensor, "pe", False)
        self.ACT = Eng(nc, es, nc.scalar, "act", True)
        self.DVE = Eng(nc, es, nc.vector, "dve", True)
        self.POOL = Eng(nc, es, nc.gpsimd, "pool", True)
        self.SP = Eng(nc, es, nc.sync, "sp", False)
        self.n_inst = 0
        self._uid = 0
        self.pes = []
        self.dsems = []

    def uid(self, p):
        self._uid += 1
        return "%s%d" % (p, self._uid)

    def sb(self, shape, dt, name=None):
        es = self.pes[-1] if self.pes else self.es
        return es.enter_context(self.nc.sbuf_tensor(self.uid(name or "sb"), list(shape), dt))

    def barrier(self):
        srcs = [self.PE, self.ACT, self.DVE, self.POOL, self.SP] + self.dsems
        for E in (self.PE, self.ACT, self.DVE, self.POOL, self.SP):
            assert not E.pending
            for S in srcs:
                if S is E or S.cnt == 0:
                    continue
                if E.seen.get(S, 0) < S.cnt:
                    E.eng.wait_ge(S.sem, S.cnt)
                    E.seen[S] = S.cnt

    @contextlib.contextmanager
    def phase(self):
        self.pes.append(contextlib.ExitStack())
        try:
            yield
        finally:
            self.barrier()
            self.pes.pop().close()

    def ps(self, shape, dt, name=None):
        return self.es.enter_context(self.nc.psum_tensor(name or self.uid("ps"), list(shape), dt))

    def dsem(self, name=None):
        d = DSem(self.nc, self.es, name or self.uid("d"))
        self.dsems.append(d)
        return d

    def _waits(self, E, reads, writes):
        need = {}
        for t in reads:
            if t.w is not None:
                _add(need, t.w)
        for t in writes:
            if t.w is not None:
                _add(need, t.w)
            for s, v in t.r.items():
                _add(need, (s, v))
        for src, val in need.items():
            if src is E and not E.self_raw:
                continue
            if E.seen.get(src, 0) < val:
                E.eng.wait_ge(src.sem, val)
                E.seen[src] = val

    def op(self, E, fn, reads=(), writes=(), inc=True):
        self._waits(E, reads, writes)
        inst = fn(E.eng)
        self.n_inst += 1
        if inc:
            E.cnt += 1
            inst.then_inc(E.sem, 1)
            mark = (E, E.cnt)
            E.pending = False
        else:
            mark = (E, E.cnt + 1)
            E.pending = True
        for t in reads:
            if t.r.get(E, 0) < mark[1]:
                t.r[E] = mark[1]
        for t in writes:
            t.w = mark
            t.r = {}
        return inst

    def dma(self, Q, ds, out, in_, reads=(), writes=()):
        self._waits(Q, reads, writes)
        inst = Q.eng.dma_start(out=out, in_=in_)
        self.n_inst += 1
        ds.cnt += 16
        inst.then_inc(ds.sem, 16)
        mark = (ds, ds.cnt)
        for t in reads:
            if t.r.get(ds, 0) < mark[1]:
                t.r[ds] = mark[1]
        for t in writes:
            t.w = mark
            t.r = {}
        return inst

    def finish(self, res_list):
        self._waits(self.SP, res_list, res_list)


class Prog:
    def __init__(self, layers=(0, 1, 2, 3), skip_mixer=False, skip_ffn=False):
        self.k = K()
        k = self.k
        nc = k.nc
        self.nc = nc
        self.layers = layers
        self.skip_mixer = skip_mixer
        self.skip_ffn = skip_ffn
        self.inputs = {}
        self.x_in = self.inp("x_in", [T, D], F32)
        self.c2 = self.inp("c2", [128, NB, 2], F32)
        self.out = nc.dram_tensor("out", [SEQ, D], F32, kind="ExternalOutput").ap()
        self.XT = nc.dram_tensor("XT", [D, T], F32).ap()
        self.XTv = self.XT.rearrange("(kc p) t -> p kc t", p=128)
        self.rXT = {}
        for i in range(T // 256):
            self.rXT[i] = Res()
        self.bank = [k.ps([128, 512], F32, "bank%d" % i) for i in range(8)]
        self.rbank = [Res() for _ in range(8)]
        self.dq = {}
        self.consts()
        self.mods = {}
        for li in layers:
            self.mods[li] = (k.sb([128, 64], F32, "vec%d" % li), k.sb([128, 48, 2], F32, "mod%d" % li), k.sb([128, 2, NB, 2], F32, "gs%d" % li))

    def inp(self, name, shape, dt):
        if name in self.inputs:
            return self.inputs[name]
        ap = self.nc.dram_tensor(name, list(shape), dt, kind="ExternalInput").ap()
        self.inputs[name] = ap
        return ap

    def ds(self, name):
        if name not in self.dq:
            self.dq[name] = self.k.dsem(name)
        return self.dq[name]

    def xt_res(self, t0, n):
        return [self.rXT[i] for i in range(t0 // 256, (t0 + n + 255) // 256)]

    def consts(self):
        k = self.k
        self.ident = k.sb([128, 128], F32, "ident")
        self.rident = Res()
        k.op(k.POOL, lambda e: e.memset(self.ident[:], 0.0), writes=[self.rident])
        k.op(k.POOL, lambda e: e.affine_select(out=self.ident[:], in_=self.ident[:], pattern=[[-1, 128]],
                                                compare_op=ALU.not_equal, fill=1.0, base=0, channel_multiplier=1),
             reads=[self.rident], writes=[self.rident])
        self.ones_bf = k.sb([128, 128], BF16, "ones_bf")
        self.rones = Res()
        k.op(k.POOL, lambda e: e.memset(self.ones_bf[:], 1.0), writes=[self.rones])
        self.sel = k.sb([8, NE, 128], F32, "sel")
        self.rsel = Res()
        k.op(k.POOL, lambda e: e.memset(self.sel[:], 0.0), writes=[self.rsel])
        k.op(k.POOL, lambda e: e.affine_select(out=self.sel[:], in_=self.sel[:], pattern=[[1, NE], [0, 128]],
                                                compare_op=ALU.not_equal, fill=1.0, base=0, channel_multiplier=-1),
             reads=[self.rsel], writes=[self.rsel])
        self.c2s = k.sb([128, NB, 2], F32, "c2s")
        self.rc2 = Res()
        k.dma(k.SP, self.ds("c"), self.c2s[:], self.c2, writes=[self.rc2])
        self.sc = k.sb([128, NB, 2], BF16, "silu_c")
        self.rsc = Res()
        k.op(k.ACT, lambda e: e.activation(out=self.sc[:], in_=self.c2s[:], func=AF.Silu), reads=[self.rc2], writes=[self.rsc])

    def load_input(self):
        k = self.k
        xin = [k.sb([128, D], F32, "xin%d" % i) for i in range(2)]
        rxin = [Res(), Res()]
        xo = [k.sb([128, NB, 512], F32, "xo%d" % i) for i in range(2)]
        rxo = [Res(), Res()]
        ti = 0
        blocks = [(0, 256)] + [(256 + i * 512, 512) for i in range(8)]
        for bi, (t0, n) in enumerate(blocks):
            o = xo[bi % 2]
            ro = rxo[bi % 2]
            for s in range(n // 128):
                xi = xin[ti % 2]
                rxi = rxin[ti % 2]
                k.dma(k.SP, self.ds("xin%d" % (ti % 2)), xi[:], self.x_in[t0 + s * 128:t0 + (s + 1) * 128, :], writes=[rxi])
                for half in range(2):
                    b = 6 + half
                    for q in range(4):
                        kc = half * 4 + q
                        k.op(k.PE, lambda e, kc=kc, q=q, b=b: e.transpose(self.bank[b][:, q * 128:(q + 1) * 128], xi[:, kc * 128:(kc + 1) * 128], self.ident[:]),
                             reads=[rxi, self.rident], writes=[self.rbank[b]])
                    eng = k.ACT if half == 0 else k.DVE
                    if half == 0:
                        k.op(k.ACT, lambda e, b=b, half=half: e.copy(out=o[:, half * 4:(half + 1) * 4, s * 128:(s + 1) * 128],
                                                                      in_=self.bank[b][:].rearrange("p (q t) -> p q t", q=4)),
                             reads=[self.rbank[b]], writes=[ro])
                    else:
                        k.op(k.DVE, lambda e, b=b, half=half: e.tensor_copy(out=o[:, half * 4:(half + 1) * 4, s * 128:(s + 1) * 128],
                                                                             in_=self.bank[b][:].rearrange("p (q t) -> p q t", q=4)),
                             reads=[self.rbank[b]], writes=[ro])
                ti += 1
            k.dma(k.SP, self.ds("xo%d" % (bi % 2)), self.XTv[:, :, t0:t0 + n], o[:, :, 0:n], reads=[ro], writes=self.xt_res(t0, n))

    def store_output(self):
        k = self.k
        xi = [k.sb([128, NB, 512], F32, "so_in%d" % i) for i in range(2)]
        rxi = [Res(), Res()]
        xo = [k.sb([128, D], F32, "so_out%d" % i) for i in range(2)]
        rxo = [Res(), Res()]
        ti = 0
        for bi in range(8):
            t0 = 256 + bi * 512
            a = xi[bi % 2]
            ra = rxi[bi % 2]
            k.dma(k.SP, self.ds("so_in%d" % (bi % 2)), a[:], self.XTv[:, :, t0:t0 + 512], reads=self.xt_res(t0, 512), writes=[ra])
            for s in range(4):
                o = xo[ti % 2]
                ro = rxo[ti % 2]
                for half in range(2):
                    b = 6 + half
                    for q in range(4):
                        kc = half * 4 + q
                        k.op(k.PE, lambda e, kc=kc, q=q, b=b: e.transpose(self.bank[b][:, q * 128:(q + 1) * 128], a[:, kc, s * 128:(s + 1) * 128], self.ident[:]),
                             reads=[ra, self.rident], writes=[self.rbank[b]])
                    if half == 0:
                        k.op(k.ACT, lambda e, b=b: e.copy(out=o[:, 0:512], in_=self.bank[b][:]), reads=[self.rbank[b]], writes=[ro])
                    else:
                        k.op(k.DVE, lambda e, b=b: e.tensor_copy(out=o[:, 512:1024], in_=self.bank[b][:]), reads=[self.rbank[b]], writes=[ro])
                r0 = bi * 512 + s * 128
                k.dma(k.SP, self.ds("so_out%d" % (ti % 2)), self.out[r0:r0 + 128, :], o[:], reads=[ro])
                self.out_res.append(ro)
                ti += 1

    def modulation(self, li):
        k = self.k
        aw = self.inp("l%d_ada_w" % li, [6, 128, NB, D], F32)
        vec = self.inp("l%d_vec" % li, [128, 64], F32)
        self.aw_slot = [k.sb([128, NB, D], BF16, "aw_slot%d" % i) for i in range(2)]
        self.raw_slot = [Res(), Res()]
        vs, mod, gs = self.mods[li]
        rvs = Res()
        k.dma(k.SP, self.ds("vec"), vs[:], vec, writes=[rvs])
        rmod = Res()
        b = 7
        for m in range(6):
            sl = self.aw_slot[m % 2]
            rsl = self.raw_slot[m % 2]
            k.dma(k.POOL, self.ds("aw%d" % (m % 2)), sl[:], aw[m], writes=[rsl])
            for oc in range(8):
                col = (m * 8 + oc) * 2
                for kc in range(8):
                    k.op(k.PE, lambda e, kc=kc, oc=oc, col=col: e.matmul(self.bank[b][:, col:col + 2], sl[:, kc, oc * 128:(oc + 1) * 128],
                                                                          self.sc[:, kc, :], start=(kc == 0), stop=(kc == 7)),
                         reads=[rsl, self.rsc], writes=[self.rbank[b]], inc=(kc == 7))
        k.op(k.DVE, lambda e: e.tensor_tensor(out=mod[:], in0=self.bank[b][:, 0:96].rearrange("p (a j) -> p a j", j=2),
                                              in1=vs[:, 0:48].unsqueeze(2).broadcast_to([128, 48, 2]), op=ALU.add),
             reads=[self.rbank[b], rvs], writes=[rmod])
        for w in range(2):
            sc_ = mod[:, 8 + 24 * w:16 + 24 * w, :]
            g_ = vs[:, 48 + 8 * w:56 + 8 * w].unsqueeze(2).broadcast_to([128, NB, 2])
            k.op(k.DVE, lambda e, w=w, sc_=sc_, g_=g_: e.scalar_tensor_tensor(out=gs[:, w], in0=sc_, scalar=1.0, in1=g_, op0=ALU.add, op1=ALU.mult),
                 reads=[rmod, rvs], writes=[rmod])
        self.mod = mod
        self.gs = gs
        self.rmod = rmod
        self.f_gs = lambda w, kc, j: gs[:, w, kc, j:j + 1]
        self.f_sh = lambda w, kc, j: mod[:, 24 * w + kc, j:j + 1]
        self.f_gt = lambda w, kc, j: mod[:, 16 + 24 * w + kc, j:j + 1]

    def prep_alloc(self, w=512):
        k = self.k
        self.sq = k.sb([128, NB, w], BF16, "sq")
        self.rsq = Res()
        self.rstd = k.sb([128, w], F32, "rstd")
        self.rrstd = Res()
        self.ptmp = [k.sb([128, w], F32, "ptmp%d" % i) for i in range(2)]
        self.rptmp = [Res(), Res()]

    def prep_h(self, xg, rxg, off, n, hT, rhT, hoff, w, j):
        a, b = self.prep_h_stages(xg, rxg, off, n, hT, rhT, hoff, w, j)
        a()
        b()

    def prep_h_stages(self, xg, rxg, off, n, hT, rhT, hoff, w, j, sq=None, rsq=None):
        k = self.k
        b = 6
        sq = self.sq if sq is None else sq
        rsq = self.rsq if rsq is None else rsq

        def st_a():
            for kc in range(NB):
                k.op(k.ACT, lambda e, kc=kc: e.activation(out=sq[:, kc, 0:n], in_=xg[:, kc, off:off + n], func=AF.Square),
                     reads=[rxg], writes=[rsq])

        def st_b():
            for kc in range(NB):
                k.op(k.PE, lambda e, kc=kc: e.matmul(self.bank[b][:, 0:n], self.ones_bf[:], sq[:, kc, 0:n], start=(kc == 0), stop=(kc == NB - 1)),
                     reads=[rsq, self.rones], writes=[self.rbank[b]], inc=(kc == NB - 1))
            k.op(k.ACT, lambda e: e.activation(out=self.rstd[:, 0:n], in_=self.bank[b][:, 0:n], func=AF.Ln, bias=EPS, scale=1.0 / D),
                 reads=[self.rbank[b]], writes=[self.rrstd])
            k.op(k.ACT, lambda e: e.activation(out=self.rstd[:, 0:n], in_=self.rstd[:, 0:n], func=AF.Exp, scale=-0.5),
                 reads=[self.rrstd], writes=[self.rrstd])
            for kc in range(NB):
                tmp = self.ptmp[kc % 2]
                rtmp = self.rptmp[kc % 2]
                k.op(k.DVE, lambda e, kc=kc, tmp=tmp: e.scalar_tensor_tensor(out=tmp[:, 0:n], in0=xg[:, kc, off:off + n], scalar=self.f_gs(w, kc, j),
                                                                           in1=self.rstd[:, 0:n], op0=ALU.mult, op1=ALU.mult),
                     reads=[rxg, self.rrstd, self.rmod], writes=[rtmp])
                k.op(k.ACT, lambda e, kc=kc, tmp=tmp: e.activation(out=hT[:, kc, hoff:hoff + n], in_=tmp[:, 0:n], func=AF.Identity,
                                                                    bias=self.f_sh(w, kc, j), scale=1.0),
                     reads=[rtmp, self.rmod], writes=[rhT])
        return st_a, st_b

    def ffn_alloc(self):
        k = self.k
        self.prep_alloc()
        self.f_hT = k.sb([128, NB, 1024], BF16, "f_hT")
        self.r_hT = Res()
        self.f_xg = [k.sb([128, NB, 1024], F32, "f_xg%d" % i) for i in range(2)]
        self.r_xg = [Res(), Res()]
        self.f_aT = k.sb([128, 28, 1024], BF16, "f_aT")
        self.r_aT = [Res(), Res()]
        self.f_wgu = [k.sb([128, NB, 256], BF16, "f_wgu%d" % i) for i in range(3)]
        self.r_wgu = [Res() for _ in range(3)]
        self.f_wd = [k.sb([128, 28, 128], BF16, "f_wd%d" % i) for i in range(2)]
        self.r_wd = [Res() for _ in range(2)]
        self.f_sg = [k.sb([128, 512], BF16, "f_sg%d" % i) for i in range(2)]
        self.r_sg = [Res(), Res()]
        self.f_gate = [k.sb([128, 1024], F32, "f_gate%d" % i) for i in range(2)]
        self.r_gate = [Res(), Res()]
        self.f_gT = k.sb([8, 1024], F32, "f_gT")
        self.r_gT = Res()
        self.f_ytmp = [k.sb([128, 512], F32, "f_ytmp%d" % i) for i in range(2)]
        self.r_ytmp = [Res(), Res()]
        self.f_rw = k.sb([128, NB, NE], BF16, "f_rw")
        self.r_rw = Res()
        self.f_rb = k.sb([128, NE], F32, "f_rb")
        self.r_rb = Res()
        self.f_lg = k.sb([128, 2, 8, NE], F32, "f_lg")
        self.r_lg = [Res(), Res()]

    def ffn(self, li, moe, need_ctx):
        k = self.k
        self.ffn_alloc()
        ne = NE if moe else 1
        pre = "l%d_" % li
        wgu = self.inp(pre + "w_gu", [ne, 28, 128, NB, 256], F32)
        wd = self.inp(pre + "w_down", [ne, 8, 128, 28, 128], F32)
        if moe:
            rw = self.inp(pre + "router", [128, NB, NE], F32)
            rb = self.inp(pre + "router_b", [1, NE], F32)
            k.dma(k.POOL, self.ds("rw"), self.f_rw[:], rw, writes=[self.r_rw])
            k.dma(k.SP, self.ds("rb"), self.f_rb[:], rb.partition_broadcast(128), writes=[self.r_rb])
        groups = ([(0, 256, 1)] if need_ctx else []) + [(256 + i * 1024, 1024, 0) for i in range(4)]
        loads = []
        for gi in range(len(groups)):
            for e in range(ne):
                for jj in range(28):
                    loads.append(("gu", e, jj))
                for c in range(8):
                    loads.append(("d", e, c))
        st = {"issued": 0, "ngu": 0, "nd": 0}
        slot_of = {}

        def issue_to(idx):
            while st["issued"] <= idx and st["issued"] < len(loads):
                i = st["issued"]
                kind, e, q = loads[i]
                if kind == "gu":
                    s = st["ngu"] % 3
                    st["ngu"] += 1
                    k.dma(k.POOL, self.ds("wgu%d" % s), self.f_wgu[s][:], wgu[e, q], writes=[self.r_wgu[s]])
                else:
                    s = st["nd"] % 2
                    st["nd"] += 1
                    k.dma(k.POOL, self.ds("wd%d" % s), self.f_wd[s][:], wd[e, q], writes=[self.r_wd[s]])
                slot_of[i] = s
                st["issued"] += 1

        li_ptr = 0
        hT, rhT, aT = self.f_hT, self.r_hT, self.f_aT

        def subs_of(n_):
            return [(s_ * 512, min(512, n_ - s_ * 512)) for s_ in range((n_ + 511) // 512)]

        def load_xg(gi_):
            t0_, n_, jc_ = groups[gi_]
            k.dma(k.SP, self.ds("f_xg%d" % (gi_ % 2)), self.f_xg[gi_ % 2][:, :, 0:n_], self.XTv[:, :, t0_:t0_ + n_], reads=self.xt_res(t0_, n_), writes=[self.r_xg[gi_ % 2]])

        def pre_stages(gi_):
            t0_, n_, jc_ = groups[gi_]
            xg_, rxg_ = self.f_xg[gi_ % 2], self.r_xg[gi_ % 2]
            st_ = []
            ab = [self.prep_h_stages(xg_, rxg_, so_, sn_, hT, rhT, so_, 1, jc_) for (so_, sn_) in subs_of(n_)]
            for a_, b_ in ab:
                st_.append(a_)
                st_.append(b_)
            if moe:
                parts = self.router_parts(n_)
                prev_b = None
                for pa_, pb_ in parts:
                    def th(pa_=pa_, prev_b=prev_b):
                        pa_()
                        if prev_b is not None:
                            prev_b()
                    st_.append(th)
                    prev_b = pb_
                st_.append(prev_b)
            return st_

        load_xg(0)
        for th in pre_stages(0):
            th()
        for gi, (t0, n, jc) in enumerate(groups):
            subs = subs_of(n)
            xg, rxg = self.f_xg[gi % 2], self.r_xg[gi % 2]
            if gi + 1 < len(groups):
                load_xg(gi + 1)
                nxt = pre_stages(gi + 1)
            else:
                nxt = []
            for e in range(ne):
                if moe:
                    gsl = e % 2
                    for (so, sn) in subs:
                        k.op(k.PE, lambda e_, so=so, sn=sn: e_.matmul(self.bank[7][:, 0:sn], self.sel[:, e, :], self.f_gT[:, so:so + sn], start=True, stop=True),
                             reads=[self.rsel, self.r_gT], writes=[self.rbank[7]])
                        k.op(k.ACT, lambda e_, so=so, sn=sn: e_.copy(out=self.f_gate[gsl][:, so:so + sn], in_=self.bank[7][:, 0:sn]),
                             reads=[self.rbank[7]], writes=[self.r_gate[gsl]])
                for jj in range(28):
                    issue_to(li_ptr + 2)
                    s = slot_of[li_ptr]
                    li_ptr += 1
                    w = self.f_wgu[s]
                    rw_ = self.r_wgu[s]
                    for si, (so, sn) in enumerate(subs):
                        bg, bu = 0 + si, 2 + si
                        for kc in range(NB):
                            k.op(k.PE, lambda e_, kc=kc, bg=bg, so=so, sn=sn: e_.matmul(self.bank[bg][:, 0:sn], w[:, kc, 0:128], hT[:, kc, so:so + sn],
                                                                                        start=(kc == 0), stop=(kc == NB - 1)),
                                 reads=[rw_, rhT], writes=[self.rbank[bg]], inc=(kc == NB - 1))
                        for kc in range(NB):
                            k.op(k.PE, lambda e_, kc=kc, bu=bu, so=so, sn=sn: e_.matmul(self.bank[bu][:, 0:sn], w[:, kc, 128:256], hT[:, kc, so:so + sn],
                                                                                        start=(kc == 0), stop=(kc == NB - 1)),
                                 reads=[rw_, rhT], writes=[self.rbank[bu]], inc=(kc == NB - 1))
                        sg = self.f_sg[si]
                        k.op(k.ACT, lambda e_, bg=bg, sn=sn, sg=sg: e_.activation(out=sg[:, 0:sn], in_=self.bank[bg][:, 0:sn], func=AF.Silu),
                             reads=[self.rbank[bg]], writes=[self.r_sg[si]])
                        k.op(k.DVE, lambda e_, bu=bu, sn=sn, so=so, sg=sg, jj=jj: e_.tensor_tensor(out=aT[:, jj, so:so + sn], in0=self.bank[bu][:, 0:sn], in1=sg[:, 0:sn], op=ALU.mult),
                             reads=[self.rbank[bu], self.r_sg[si]], writes=[self.r_aT[si]])
                for c in range(8):
                    if e == ne - 1 and nxt:
                        per = (len(nxt) + 7 - c) // (8 - c)
                        for _ in range(per):
                            nxt.pop(0)()
                    issue_to(li_ptr + 1)
                    s = slot_of[li_ptr]
                    li_ptr += 1
                    w = self.f_wd[s]
                    rw_ = self.r_wd[s]
                    for si, (so, sn) in enumerate(subs):
                        by = 4 + si
                        for jj in range(28):
                            k.op(k.PE, lambda e_, jj=jj, by=by, so=so, sn=sn: e_.matmul(self.bank[by][:, 0:sn], w[:, jj, :], aT[:, jj, so:so + sn],
                                                                                        start=(jj == 0), stop=(jj == 27)),
                                 reads=[rw_, self.r_aT[si]], writes=[self.rbank[by]], inc=(jj == 27))
                        if moe:
                            yt = self.f_ytmp[si]
                            k.op(k.DVE, lambda e_, by=by, so=so, sn=sn, yt=yt: e_.tensor_tensor(out=yt[:, 0:sn], in0=self.bank[by][:, 0:sn], in1=self.f_gate[gsl][:, so:so + sn], op=ALU.mult),
                                 reads=[self.rbank[by], self.r_gate[gsl]], writes=[self.r_ytmp[si]])
                            k.op(k.DVE, lambda e_, c=c, so=so, sn=sn, yt=yt: e_.scalar_tensor_tensor(out=xg[:, c, so:so + sn], in0=yt[:, 0:sn], scalar=self.f_gt(1, c, jc),
                                                                                                    in1=xg[:, c, so:so + sn], op0=ALU.mult, op1=ALU.add),
                                 reads=[self.r_ytmp[si], self.rmod, rxg], writes=[rxg])
                        else:
                            k.op(k.DVE, lambda e_, c=c, by=by, so=so, sn=sn: e_.scalar_tensor_tensor(out=xg[:, c, so:so + sn], in0=self.bank[by][:, 0:sn], scalar=self.f_gt(1, c, jc),
                                                                                                    in1=xg[:, c, so:so + sn], op0=ALU.mult, op1=ALU.add),
                                 reads=[self.rbank[by], self.rmod, rxg], writes=[rxg])
            k.dma(k.SP, self.ds("f_xo"), self.XTv[:, :, t0:t0 + n], xg[:, :, 0:n], reads=[rxg], writes=self.xt_res(t0, n))

    def router(self, n):
        for pa, pb in self.router_parts(n):
            pa()
            pb()

    def router_parts(self, n):
        parts = []
        for ti in range(n // 128):
            parts.append(self._router_tile(ti))
        return parts

    def _router_tile(self, ti):
        k = self.k
        hT, rhT = self.f_hT, self.r_hT
        b = 7
        tsl = slice(ti * 128, (ti + 1) * 128)
        lg = self.f_lg[:, ti % 2]
        rl = self.r_lg[ti % 2]

        L, m1, e1, L2, m2, e2, dd, g_ = (lg[:, i, :] for i in range(8))

        def part_a():
            for kc in range(NB):
                k.op(k.PE, lambda e, kc=kc: e.matmul(self.bank[b][:, 0:NE], hT[:, kc, tsl], self.f_rw[:, kc, :], start=(kc == 0), stop=(kc == NB - 1)),
                     reads=[rhT, self.r_rw], writes=[self.rbank[b]], inc=(kc == NB - 1))
            k.op(k.DVE, lambda e: e.tensor_tensor(out=L, in0=self.bank[b][:, 0:NE], in1=self.f_rb[:], op=ALU.add), reads=[self.rbank[b], self.r_rb], writes=[rl])
            k.op(k.DVE, lambda e: e.reduce_max(out=m1[:, 0:1], in_=L, axis=AX.X), reads=[rl], writes=[rl])
            k.op(k.DVE, lambda e: e.tensor_scalar(out=e1, in0=L, scalar1=m1[:, 0:1], scalar2=None, op0=ALU.is_equal), reads=[rl], writes=[rl])
            k.op(k.DVE, lambda e: e.scalar_tensor_tensor(out=L2, in0=e1, scalar=-1e30, in1=L, op0=ALU.mult, op1=ALU.add), reads=[rl], writes=[rl])
            k.op(k.DVE, lambda e: e.reduce_max(out=m2[:, 0:1], in_=L2, axis=AX.X), reads=[rl], writes=[rl])
            k.op(k.DVE, lambda e: e.tensor_scalar(out=e2, in0=L2, scalar1=m2[:, 0:1], scalar2=None, op0=ALU.is_equal), reads=[rl], writes=[rl])
            k.op(k.DVE, lambda e: e.tensor_tensor(out=dd[:, 0:1], in0=m2[:, 0:1], in1=m1[:, 0:1], op=ALU.subtract), reads=[rl], writes=[rl])
            k.op(k.ACT, lambda e: e.activation(out=dd[:, 1:2], in_=dd[:, 0:1], func=AF.Sigmoid), reads=[rl], writes=[rl])
            k.op(k.DVE, lambda e: e.tensor_scalar(out=dd[:, 2:3], in0=dd[:, 1:2], scalar1=-1.0, scalar2=1.0, op0=ALU.mult, op1=ALU.add), reads=[rl], writes=[rl])
            k.op(k.DVE, lambda e: e.tensor_scalar(out=g_, in0=e1, scalar1=dd[:, 2:3], scalar2=None, op0=ALU.mult), reads=[rl], writes=[rl])
            k.op(k.DVE, lambda e: e.scalar_tensor_tensor(out=g_, in0=e2, scalar=dd[:, 1:2], in1=g_, op0=ALU.mult, op1=ALU.add), reads=[rl], writes=[rl])

        def part_b():
            k.op(k.PE, lambda e: e.transpose(self.bank[b][0:8, 128:256], g_, self.ident[:]), reads=[rl, self.rident], writes=[self.rbank[b]])
            k.op(k.ACT, lambda e: e.copy(out=self.f_gT[:, tsl], in_=self.bank[b][0:8, 128:256]), reads=[self.rbank[b]], writes=[self.r_gT])
        return part_a, part_b


def _fm(v, nchunk):
    return np.ascontiguousarray(np.asarray(v, np.float32).reshape(nchunk, 128).T)


def host_weights(inp, layers):
    w = {}
    for li in layers:
        p = "l%d_" % li
        aw = np.asarray(inp[p + "ada_w"], np.float32)
        w[p + "ada_w"] = np.ascontiguousarray(aw.reshape(NB, 128, 6, D).transpose(2, 1, 0, 3))
        vec = np.zeros((128, 64), np.float32)
        vec[:, 0:48] = _fm(inp[p + "ada_b"], 48)
        vec[:, 48:56] = _fm(inp[p + "norm_mix"], 8)
        vec[:, 56:64] = _fm(inp[p + "norm_ffn"], 8)
        w[p + "vec"] = vec
        if li % 3 == 0:
            w.update(rope_consts())
            w[p + "wqkv"] = np.ascontiguousarray(np.asarray(inp[p + "wqkv"], np.float32).reshape(NB, 128, 1536).transpose(1, 0, 2))
            w[p + "wo"] = np.ascontiguousarray(np.asarray(inp[p + "wo"], np.float32).reshape(NH, 64, D).transpose(1, 0, 2))
            w[p + "qkn"] = np.ascontiguousarray(np.tile(np.stack([np.asarray(inp[p + "q_norm"], np.float32), np.asarray(inp[p + "k_norm"], np.float32)], axis=1), (2, 1)))
        if li % 3 == 1:
            w.update(ssd_consts())
            ip = np.asarray(inp[p + "ssm_in_proj"], np.float32)
            fm3 = lambda a: np.ascontiguousarray(a.reshape(NB, 128, a.shape[1]).transpose(1, 0, 2))
            w[p + "s_wx"] = fm3(ip[:, 2048:])
            w[p + "s_wz"] = fm3(ip[:, :2048])
            w[p + "s_wo"] = np.ascontiguousarray(np.asarray(inp[p + "ssm_out_proj"], np.float32).reshape(16, 128, D).transpose(1, 0, 2))
            w[p + "s_cw"] = np.ascontiguousarray(np.asarray(inp[p + "ssm_conv_w"], np.float32).reshape(5, NXC, 128).transpose(2, 1, 0))
            w[p + "s_cbf"] = _fm(inp[p + "ssm_conv_b"], NXC)
            w[p + "s_cbr"] = np.asarray(inp[p + "ssm_conv_b"], np.float32).reshape(1, 3072)
            w[p + "s_vec"] = np.concatenate([np.asarray(inp[p + n_], np.float32) for n_ in ("ssm_a_log_fwd", "ssm_a_log_bwd", "ssm_dt_bias_fwd", "ssm_dt_bias_bwd", "ssm_d_skip")]).reshape(1, 160)
            w[p + "s_onorm"] = np.asarray(inp[p + "ssm_out_norm"], np.float32).reshape(1, SI)
        if li % 3 == 2:
            w[p + "sc_in"] = np.ascontiguousarray(np.asarray(inp[p + "sc_in_proj"], np.float32).reshape(NB, 128, 3 * D).transpose(1, 0, 2))
            w[p + "sc_out"] = np.ascontiguousarray(np.asarray(inp[p + "sc_out_proj"], np.float32).reshape(NB, 128, D).transpose(1, 0, 2))
            w[p + "sc_cw"] = np.ascontiguousarray(np.asarray(inp[p + "sc_conv_w"], np.float32).reshape(3, NB, 128).transpose(2, 1, 0))
        if li % 2 == 0:
            gu = np.asarray(inp[p + "ffn_w_gu"], np.float32)[None]
            dn = np.asarray(inp[p + "ffn_w_down"], np.float32)[None]
        else:
            gu = np.asarray(inp[p + "moe_w_gu"], np.float32)
            dn = np.asarray(inp[p + "moe_w_down"], np.float32)
            w[p + "router"] = np.ascontiguousarray(np.asarray(inp[p + "moe_router"], np.float32).reshape(NB, 128, NE).transpose(1, 0, 2))
            w[p + "router_b"] = np.asarray(inp[p + "moe_router_b"], np.float32).reshape(1, NE)
        ne = gu.shape[0]
        w[p + "w_gu"] = np.ascontiguousarray(gu.reshape(ne, NB, 128, 2, 28, 128).transpose(0, 4, 2, 1, 3, 5)).reshape(ne, 28, 128, NB, 256)
        w[p + "w_down"] = np.ascontiguousarray(dn.reshape(ne, 28, 128, 8, 128).transpose(0, 3, 2, 1, 4))
    return w


def host_core_inputs(inp, b):
    x_in = np.concatenate([np.asarray(inp["ctx"][b], np.float32), np.asarray(inp["x"][b], np.float32)], axis=0)
    c2 = np.stack([_fm(inp["c"][b], NB), _fm(inp["c_ctx"], NB)], axis=-1)
    return {"x_in": np.ascontiguousarray(x_in), "c2": np.ascontiguousarray(c2)}


def build(layers=(0, 1, 2, 3), skip_mixer=False, skip_ffn=False):
    P = Prog(layers, skip_mixer, skip_ffn)
    k = P.k
    P.out_res = []
    with k.phase():
        P.load_input()
    for li in layers:
        with k.phase():
            P.modulation(li)
        if not skip_mixer:
            with k.phase():
                P.mixer(li)
        if not skip_ffn:
            with k.phase():
                P.ffn(li, moe=(li % 2 == 1), need_ctx=(li < 3))
    with k.phase():
        P.store_output()
    for name in ("so_out0", "so_out1"):
        d = P.dq[name]
        k.SP.eng.wait_ge(d.sem, d.cnt)
    k.es.close()
    return P


def run(inp, cores=range(8), trace=False, **kw):
    P = build(**kw)
    w = host_weights(inp, P.layers)
    in_maps = []
    for b in cores:
        m = dict(w)
        m.update(host_core_inputs(inp, b))
        m = {kk: vv for kk, vv in m.items() if kk in P.inputs}
        assert set(m) == set(P.inputs), (set(P.inputs) - set(m))
        in_maps.append(m)
    if trace:
        res = run_bass_kernel_spmd(P.nc, in_maps, core_ids=list(range(len(in_maps))), trace=True)
        print("EXEC_TIME_NS", res.exec_time_ns)
    else:
        res = run_bass_kernel_spmd(P.nc, in_maps, core_ids=list(range(len(in_maps))))
    return np.stack([r["out"] for r in res.results], axis=0)


def kernel(x, c, ctx, c_ctx,
           l0_ada_w, l0_ada_b, l0_norm_mix, l0_norm_ffn, l0_wqkv, l0_q_norm, l0_k_norm, l0_wo,
           l0_ffn_w_gu, l0_ffn_w_down,
           l1_ada_w, l1_ada_b, l1_norm_mix, l1_norm_ffn, l1_ssm_in_proj, l1_ssm_conv_w, l1_ssm_conv_b,
           l1_ssm_a_log_fwd, l1_ssm_a_log_bwd, l1_ssm_dt_bias_fwd, l1_ssm_dt_bias_bwd, l1_ssm_d_skip,
           l1_ssm_out_norm, l1_ssm_out_proj, l1_moe_router, l1_moe_router_b, l1_moe_w_gu, l1_moe_w_down,
           l2_ada_w, l2_ada_b, l2_norm_mix, l2_norm_ffn, l2_sc_in_proj, l2_sc_conv_w, l2_sc_out_proj,
           l2_ffn_w_gu, l2_ffn_w_down,
           l3_ada_w, l3_ada_b, l3_norm_mix, l3_norm_ffn, l3_wqkv, l3_q_norm, l3_k_norm, l3_wo,
           l3_moe_router, l3_moe_router_b, l3_moe_w_gu, l3_moe_w_down):
    inputs = dict(
        x=x, c=c, ctx=ctx, c_ctx=c_ctx,
        l0_ada_w=l0_ada_w, l0_ada_b=l0_ada_b, l0_norm_mix=l0_norm_mix, l0_norm_ffn=l0_norm_ffn, l0_wqkv=l0_wqkv,
        l0_q_norm=l0_q_norm, l0_k_norm=l0_k_norm, l0_wo=l0_wo, l0_ffn_w_gu=l0_ffn_w_gu, l0_ffn_w_down=l0_ffn_w_down,
        l1_ada_w=l1_ada_w, l1_ada_b=l1_ada_b, l1_norm_mix=l1_norm_mix, l1_norm_ffn=l1_norm_ffn,
        l1_ssm_in_proj=l1_ssm_in_proj, l1_ssm_conv_w=l1_ssm_conv_w, l1_ssm_conv_b=l1_ssm_conv_b,
        l1_ssm_a_log_fwd=l1_ssm_a_log_fwd, l1_ssm_a_log_bwd=l1_ssm_a_log_bwd, l1_ssm_dt_bias_fwd=l1_ssm_dt_bias_fwd,
        l1_ssm_dt_bias_bwd=l1_ssm_dt_bias_bwd, l1_ssm_d_skip=l1_ssm_d_skip, l1_ssm_out_norm=l1_ssm_out_norm,
        l1_ssm_out_proj=l1_ssm_out_proj, l1_moe_router=l1_moe_router, l1_moe_router_b=l1_moe_router_b,
        l1_moe_w_gu=l1_moe_w_gu, l1_moe_w_down=l1_moe_w_down,
        l2_ada_w=l2_ada_w, l2_ada_b=l2_ada_b, l2_norm_mix=l2_norm_mix, l2_norm_ffn=l2_norm_ffn,
        l2_sc_in_proj=l2_sc_in_proj, l2_sc_conv_w=l2_sc_conv_w, l2_sc_out_proj=l2_sc_out_proj,
        l2_ffn_w_gu=l2_ffn_w_gu, l2_ffn_w_down=l2_ffn_w_down,
        l3_ada_w=l3_ada_w, l3_ada_b=l3_ada_b, l3_norm_mix=l3_norm_mix, l3_norm_ffn=l3_norm_ffn, l3_wqkv=l3_wqkv,
        l3_q_norm=l3_q_norm, l3_k_norm=l3_k_norm, l3_wo=l3_wo, l3_moe_router=l3_moe_router,
        l3_moe_router_b=l3_moe_router_b, l3_moe_w_gu=l3_moe_w_gu, l3_moe_w_down=l3_moe_w_down)
    return run(inputs).astype(np.float32)


BLOCKS = [(0, 256, 1)] + [(256 + i * 512, 512, 0) for i in range(8)]


def _mixer(self, li):
    kind = li % 3
    if kind == 0:
        self.attention(li, need_ctx=(li < 3))
    elif kind == 1:
        self.ssd(li)
    else:
        self.shortconv(li)


Prog.mixer = _mixer


def _group(self, bank, n, lhs_fn, rhs_fn, nk, reads, m0=0, m1=128):
    k = self.k
    for kc in range(nk):
        k.op(k.PE, lambda e, kc=kc: e.matmul(self.bank[bank][m0:m1, 0:n], lhs_fn(kc), rhs_fn(kc), start=(kc == 0), stop=(kc == nk - 1)),
             reads=reads, writes=[self.rbank[bank]], inc=(kc == nk - 1))


Prog.group = _group


def _shortconv(self, li):
    k = self.k
    p = "l%d_" % li
    win = self.inp(p + "sc_in", [128, NB, 3 * D], F32)
    wout = self.inp(p + "sc_out", [128, NB, D], F32)
    cw = self.inp(p + "sc_cw", [128, NB, 3], F32)
    self.prep_alloc()
    Win = k.sb([128, NB, 3 * D], BF16, "sc_Win")
    Wout = k.sb([128, NB, D], BF16, "sc_Wout")
    cws = k.sb([128, NB, 3], F32, "sc_cw")
    rW = Res()
    k.dma(k.POOL, self.ds("scw"), Win[:], win, writes=[rW])
    k.dma(k.POOL, self.ds("scw"), Wout[:], wout, writes=[rW])
    k.dma(k.SP, self.ds("scc"), cws[:], cw, writes=[rW])
    U = {1: k.sb([128, NB, CTX + 2], BF16, "sc_Uc"), 0: k.sb([128, NB, SEQ + 2], BF16, "sc_Ul")}
    rU = Res()
    for j in (0, 1):
        L = SEQ if j == 0 else CTX
        k.op(k.POOL, lambda e, j=j: e.memset(U[j][:, :, 0:1], 0.0), writes=[rU])
        k.op(k.POOL, lambda e, j=j, L=L: e.memset(U[j][:, :, L + 1:L + 2], 0.0), writes=[rU])
    xg = k.sb([128, NB, 512], F32, "sc_xg")
    rxg = Res()
    hT = k.sb([128, NB, 512], BF16, "sc_hT")
    rhT = Res()
    gT = k.sb([128, NB, 512], BF16, "sc_gT")
    rgT = Res()
    ctmp = [k.sb([128, 512], F32, "sc_ct%d" % i) for i in range(2)]
    rct = [Res(), Res()]
    vt = [k.sb([128, 512], F32, "sc_vt%d" % i) for i in range(2)]
    rvt = [Res(), Res()]
    for ps in (1, 2):
        for (t0, n, jc) in BLOCKS:
            u0 = (t0 - 256 if jc == 0 else t0) + 1
            k.dma(k.SP, self.ds("sc_xg"), xg[:, :, 0:n], self.XTv[:, :, t0:t0 + n], reads=self.xt_res(t0, n), writes=[rxg])
            self.prep_h(xg, rxg, 0, n, hT, rhT, 0, 0, jc)
            if ps == 1:
                for oc in range(NB):
                    bc, bx = oc % 2, 2 + oc % 2
                    self.group(bc, n, lambda kc, oc=oc: Win[:, kc, D + oc * 128:D + (oc + 1) * 128], lambda kc: hT[:, kc, 0:n], NB, [rW, rhT])
                    self.group(bx, n, lambda kc, oc=oc: Win[:, kc, 2 * D + oc * 128:2 * D + (oc + 1) * 128], lambda kc: hT[:, kc, 0:n], NB, [rW, rhT])
                    ct = ctmp[oc % 2]
                    k.op(k.ACT, lambda e, bc=bc, ct=ct: e.copy(out=ct[:, 0:n], in_=self.bank[bc][:, 0:n]), reads=[self.rbank[bc]], writes=[rct[oc % 2]])
                    k.op(k.DVE, lambda e, bx=bx, ct=ct, oc=oc: e.tensor_tensor(out=U[jc][:, oc, u0:u0 + n], in0=self.bank[bx][:, 0:n], in1=ct[:, 0:n], op=ALU.mult),
                         reads=[self.rbank[bx], rct[oc % 2]], writes=[rU])
            else:
                for oc in range(NB):
                    bb = oc % 2
                    self.group(bb, n, lambda kc, oc=oc: Win[:, kc, oc * 128:(oc + 1) * 128], lambda kc: hT[:, kc, 0:n], NB, [rW, rhT])
                    v = vt[oc % 2]
                    rv = rvt[oc % 2]
                    k.op(k.DVE, lambda e, oc=oc, v=v: e.tensor_scalar(out=v[:, 0:n], in0=U[jc][:, oc, u0 - 1:u0 - 1 + n], scalar1=cws[:, oc, 0:1], scalar2=None, op0=ALU.mult),
                         reads=[rU, rW], writes=[rv])
                    for tap in (1, 2):
                        k.op(k.DVE, lambda e, oc=oc, v=v, tap=tap: e.scalar_tensor_tensor(out=v[:, 0:n], in0=U[jc][:, oc, u0 - 1 + tap:u0 - 1 + tap + n], scalar=cws[:, oc, tap:tap + 1],
                                                                                 in1=v[:, 0:n], op0=ALU.mult, op1=ALU.add),
                             reads=[rU, rW, rv], writes=[rv])
                    k.op(k.DVE, lambda e, oc=oc, v=v, bb=bb: e.tensor_tensor(out=gT[:, oc, 0:n], in0=self.bank[bb][:, 0:n], in1=v[:, 0:n], op=ALU.mult),
                         reads=[self.rbank[bb], rv], writes=[rgT])
                for c in range(NB):
                    by = 4 + c % 2
                    self.group(by, n, lambda kc, c=c: Wout[:, kc, c * 128:(c + 1) * 128], lambda kc: gT[:, kc, 0:n], NB, [rW, rgT])
                    k.op(k.DVE, lambda e, c=c, by=by: e.scalar_tensor_tensor(out=xg[:, c, 0:n], in0=self.bank[by][:, 0:n], scalar=self.f_gt(0, c, jc), in1=xg[:, c, 0:n],
                                                                             op0=ALU.mult, op1=ALU.add),
                         reads=[self.rbank[by], self.rmod, rxg], writes=[rxg])
                k.dma(k.SP, self.ds("sc_xo"), self.XTv[:, :, t0:t0 + n], xg[:, :, 0:n], reads=[rxg], writes=self.xt_res(t0, n))


Prog.shortconv = _shortconv


def _attention(self, li, need_ctx):
    k = self.k
    p = "l%d_" % li
    wqkv_d = self.inp(p + "wqkv", [128, NB, 1536], F32)
    wo_d = self.inp(p + "wo", [64, NH, D], F32)
    qkn_d = self.inp(p + "qkn", [128, 2], F32)
    cos_d = self.inp("rope_cos", [64, SEQ], F32)
    sin_d = self.inp("rope_sin", [64, SEQ], F32)
    R_d = self.inp("rope_R", [128, 128], F32)
    QT_d = self.nc.dram_tensor("QT%d" % li, [64, NH, T], BF16).ap()
    rQT = {i: Res() for i in range(len(BLOCKS))}
    NT = T // 128
    KT = k.sb([128, NKV, T], BF16, "a_KT")
    rKT = Res()
    VX = k.sb([128, NT, NKV, 96], BF16, "a_VX")
    rVX = Res()
    Wo = k.sb([64, NH, D], BF16, "a_Wo")
    qkn = k.sb([128, 2], F32, "a_qkn")
    onesf = k.sb([65, 64], F32, "a_onesf")
    rW = Res()
    k.dma(k.POOL, self.ds("a_w"), Wo[:], wo_d, writes=[rW])
    k.dma(k.SP, self.ds("a_c"), qkn[:], qkn_d, writes=[rW])
    k.op(k.DVE, lambda e: e.memset(onesf[:], 1.0), writes=[rW])
    k.op(k.DVE, lambda e: e.memset(VX[:, :, :, 64:96], 1.0), writes=[rVX])
    k.op(k.DVE, lambda e: e.memset(KT[64:128], 0.0), writes=[rKT])
    with k.phase():
        self.prep_alloc()
        Wqkv = k.sb([128, NB, 1536], BF16, "a_Wqkv")
        Rm = k.sb([128, 128], BF16, "a_R")
        rW1 = Res()
        k.dma(k.POOL, self.ds("a_w"), Wqkv[:], wqkv_d, writes=[rW1])
        k.dma(k.POOL, self.ds("a_w"), Rm[:], R_d, writes=[rW1])
        cs = k.sb([128, 2, 512], F32, "a_cs")
        rcs = Res()
        BD = k.sb([128, 128], BF16, "a_BD")
        k.op(k.DVE, lambda e: e.memset(BD[:], 0.0), writes=[rW1])
        k.op(k.DVE, lambda e: e.memset(BD[0:64, 0:64], 1.0), writes=[rW1])
        k.op(k.DVE, lambda e: e.memset(BD[64:128, 64:128], 1.0), writes=[rW1])
        xg = k.sb([128, NB, 512], F32, "a_xg")
        rxg = Res()
        hT = k.sb([128, NB, 512], BF16, "a_hT")
        rhT = Res()
        QTo = k.sb([128, NH // 2, 512], BF16, "a_QTo")
        rQTo = Res()
        sq = [k.sb([128, 512], BF16, "a_sq%d" % i) for i in range(2)]
        rsq = [Res(), Res()]
        rstd = [k.sb([128, 512], F32, "a_rstd%d" % i) for i in range(2)]
        rrstd = [Res(), Res()]
        qn = [k.sb([128, 512], F32, "a_qn%d" % i) for i in range(2)]
        rqn = [Res(), Res()]
        qnb = [k.sb([128, 512], BF16, "a_qnb%d" % i) for i in range(2)]
        rqnb = [Res(), Res()]
        t1 = [k.sb([128, 512], F32, "a_t1%d" % i) for i in range(2)]
        rt1 = [Res(), Res()]
        t2 = [k.sb([128, 512], F32, "a_t2%d" % i) for i in range(2)]
        rt2 = [Res(), Res()]
        QT_v = QT_d.rearrange("d (pr two) t -> d pr two t", two=2)
        for bi, (t0, n, jc) in enumerate(BLOCKS):
            k.dma(k.SP, self.ds("a_xg"), xg[:, :, 0:n], self.XTv[:, :, t0:t0 + n], reads=self.xt_res(t0, n), writes=[rxg])
            if jc == 0:
                for hf in range(2):
                    k.dma(k.SP, self.ds("a_cs"), cs[hf * 64:(hf + 1) * 64, 0, 0:n], cos_d[:, t0 - 256:t0 - 256 + n], writes=[rcs])
                    k.dma(k.SP, self.ds("a_cs"), cs[hf * 64:(hf + 1) * 64, 1, 0:n], sin_d[:, t0 - 256:t0 - 256 + n], writes=[rcs])
            self.prep_h(xg, rxg, 0, n, hT, rhT, 0, 0, jc)
            do_q = (jc == 0) or need_ctx
            units = ([("q", pr) for pr in range(NH // 2)] if do_q else []) + [("k", g) for g in range(NKV)]
            for ui, (kind_, idx_) in enumerate(units):
                isq = kind_ == "q"
                M = 128 if isq else 64
                col0 = idx_ * 128 if isq else D + idx_ * 64
                i2 = ui % 2
                bq, bs, br = i2, 2 + i2, 4 + i2
                self.group(bq, n, lambda kc, col0=col0, M=M: Wqkv[:, kc, col0:col0 + M], lambda kc: hT[:, kc, 0:n], NB, [rW1, rhT], 0, M)
                k.op(k.ACT, lambda e, bq=bq, i2=i2, M=M: e.activation(out=sq[i2][0:M, 0:n], in_=self.bank[bq][0:M, 0:n], func=AF.Square),
                     reads=[self.rbank[bq]], writes=[rsq[i2]])
                k.op(k.PE, lambda e, bs=bs, i2=i2, M=M: e.matmul(self.bank[bs][0:M, 0:n], BD[0:M, 0:M], sq[i2][0:M, 0:n], start=True, stop=True),
                     reads=[rsq[i2], rW1], writes=[self.rbank[bs]])
                k.op(k.ACT, lambda e, bs=bs, i2=i2, M=M: e.activation(out=rstd[i2][0:M, 0:n], in_=self.bank[bs][0:M, 0:n], func=AF.Ln, bias=EPS, scale=1.0 / DH),
                     reads=[self.rbank[bs]], writes=[rrstd[i2]])
                k.op(k.ACT, lambda e, i2=i2, M=M: e.activation(out=rstd[i2][0:M, 0:n], in_=rstd[i2][0:M, 0:n], func=AF.Exp, scale=-0.5),
                     reads=[rrstd[i2]], writes=[rrstd[i2]])
                gain = qkn[0:M, 0:1] if isq else qkn[0:M, 1:2]
                if isq:
                    dest, rdest = QTo[:, idx_, 0:n], rQTo
                else:
                    dest, rdest = KT[0:64, idx_, t0:t0 + n], rKT
                if jc == 1:
                    k.op(k.DVE, lambda e, bq=bq, i2=i2, dest=dest, gain=gain, M=M: e.scalar_tensor_tensor(out=dest, in0=self.bank[bq][0:M, 0:n], scalar=gain, in1=rstd[i2][0:M, 0:n],
                                                                                                      op0=ALU.mult, op1=ALU.mult),
                         reads=[self.rbank[bq], rrstd[i2], rW], writes=[rdest])
                else:
                    k.op(k.DVE, lambda e, bq=bq, i2=i2, gain=gain, M=M: e.scalar_tensor_tensor(out=qn[i2][0:M, 0:n], in0=self.bank[bq][0:M, 0:n], scalar=gain, in1=rstd[i2][0:M, 0:n],
                                                                                           op0=ALU.mult, op1=ALU.mult),
                         reads=[self.rbank[bq], rrstd[i2], rW], writes=[rqn[i2]])
                    k.op(k.ACT, lambda e, i2=i2, M=M: e.copy(out=qnb[i2][0:M, 0:n], in_=qn[i2][0:M, 0:n]), reads=[rqn[i2]], writes=[rqnb[i2]])
                    k.op(k.PE, lambda e, br=br, i2=i2, M=M: e.matmul(self.bank[br][0:M, 0:n], Rm[0:M, 0:M], qnb[i2][0:M, 0:n], start=True, stop=True),
                         reads=[rqnb[i2], rW1], writes=[self.rbank[br]])
                    k.op(k.POOL, lambda e, i2=i2, M=M: e.tensor_tensor(out=t1[i2][0:M, 0:n], in0=qn[i2][0:M, 0:n], in1=cs[0:M, 0, 0:n], op=ALU.mult),
                         reads=[rqn[i2], rcs], writes=[rt1[i2]])
                    k.op(k.DVE, lambda e, br=br, i2=i2, M=M: e.tensor_tensor(out=t2[i2][0:M, 0:n], in0=self.bank[br][0:M, 0:n], in1=cs[0:M, 1, 0:n], op=ALU.mult),
                         reads=[self.rbank[br], rcs], writes=[rt2[i2]])
                    k.op(k.POOL, lambda e, i2=i2, dest=dest, M=M: e.tensor_tensor(out=dest, in0=t1[i2][0:M, 0:n], in1=t2[i2][0:M, 0:n], op=ALU.add),
                         reads=[rt1[i2], rt2[i2]], writes=[rdest])
            for s in range(n // 128):
                ti = t0 // 128 + s
                self.group(7, 256, lambda kc, s=s: hT[:, kc, s * 128:(s + 1) * 128], lambda kc: Wqkv[:, kc, D + 256:D + 512], NB, [rW1, rhT])
                k.op(k.ACT, lambda e, ti=ti: e.copy(out=VX[:, ti, :, 0:64], in_=self.bank[7][:, 0:256].rearrange("p (g d) -> p g d", g=NKV)),
                     reads=[self.rbank[7]], writes=[rVX])
            if do_q:
                for hf in range(2):
                    k.dma(k.SP, self.ds("a_qo"), QT_v[:, :, hf, t0:t0 + n], QTo[hf * 64:(hf + 1) * 64, :, 0:n], reads=[rQTo], writes=[rQT[bi]])
    with k.phase():
        QTi = k.sb([128, NH, 512], BF16, "a_QTi")
        rQTi = Res()
        k.op(k.DVE, lambda e: e.memset(QTi[64:128], 0.0), writes=[rQTi])
        pT = [k.sb([128, 512], BF16, "a_pT%d" % i) for i in range(4)]
        rpT = [Res() for _ in range(4)]
        oT = k.sb([64, NH, 512], BF16, "a_oT")
        roT = Res()
        ob = [k.sb([64, 512], F32, "a_ob%d" % i) for i in range(2)]
        rob = [Res(), Res()]
        rs = [k.sb([65, 512], F32, "a_rs%d" % i) for i in range(2)]
        rrs = [Res(), Res()]
        xg = k.sb([128, NB, 512], F32, "a_xg2")
        rxg = Res()
        for bi, (t0, n, jc) in enumerate(BLOCKS):
            if jc == 1 and not need_ctx:
                continue
            k.dma(k.SP, self.ds("a_qi"), QTi[0:64, :, 0:n], QT_d[:, :, t0:t0 + n], reads=[rQT[bi]], writes=[rQTi])
            k.dma(k.SP, self.ds("a_xg"), xg[:, :, 0:n], self.XTv[:, :, t0:t0 + n], reads=self.xt_res(t0, n), writes=[rxg])
            ktiles = list(range(2)) if jc == 1 else list(range(NT))
            items = [(h, idx, kt) for h in range(NH) for idx, kt in enumerate(ktiles)]
            SB = [0, 1, 7]
            LOOK = 2

            def emit_s(ii):
                h_, idx_, kt_ = items[ii]
                bs_ = SB[ii % 3]
                k.op(k.PE, lambda e: e.matmul(self.bank[bs_][:, 0:n], KT[:, h_ // 4, kt_ * 128:(kt_ + 1) * 128], QTi[:, h_, 0:n], start=True, stop=True),
                     reads=[rKT, rQTi], writes=[self.rbank[bs_]])

            for ii in range(min(LOOK, len(items))):
                emit_s(ii)
            pend = []
            for ii, (h, idx, kt) in enumerate(items):
                g = h // 4
                bo = 2 + h % 2
                bs = SB[ii % 3]
                pp = ii % 4
                if ii + LOOK < len(items):
                    emit_s(ii + LOOK)
                k.op(k.ACT, lambda e, bs=bs, pp=pp: e.activation(out=pT[pp][:, 0:n], in_=self.bank[bs][:, 0:n], func=AF.Exp, scale=DH ** -0.5),
                     reads=[self.rbank[bs]], writes=[rpT[pp]])
                k.op(k.PE, lambda e, bo=bo, kt=kt, pp=pp, idx=idx, g=g: e.matmul(self.bank[bo][0:96, 0:n], VX[:, kt, g, :], pT[pp][:, 0:n],
                                                                                  start=(idx == 0), stop=(idx == len(ktiles) - 1)),
                     reads=[rVX, rpT[pp]], writes=[self.rbank[bo]])
                for (due, fa, fb) in list(pend):
                    if ii >= due:
                        fb()
                        pend.remove((due, fa, fb))
                if idx != len(ktiles) - 1:
                    continue

                def fin_a(bo=bo, h=h):
                    k.op(k.DVE, lambda e: e.reciprocal(out=rs[h % 2][64:65, 0:n], in_=self.bank[bo][64:65, 0:n]), reads=[self.rbank[bo]], writes=[rrs[h % 2]])
                    k.op(k.ACT, lambda e: e.copy(out=ob[h % 2][:, 0:n], in_=self.bank[bo][0:64, 0:n]), reads=[self.rbank[bo]], writes=[rob[h % 2]])

                def fin_b(bo=bo, h=h):
                    k.op(k.PE, lambda e: e.matmul(self.bank[4][0:64, 0:n], onesf[64:65, 0:64], rs[h % 2][64:65, 0:n], start=True, stop=True),
                         reads=[rrs[h % 2], rW], writes=[self.rbank[4]])
                    k.op(k.DVE, lambda e: e.tensor_tensor(out=oT[:, h, 0:n], in0=ob[h % 2][:, 0:n], in1=self.bank[4][0:64, 0:n], op=ALU.mult),
                         reads=[rob[h % 2], self.rbank[4]], writes=[roT])

                fin_a()
                pend.append((ii + min(12, len(ktiles) - 2), fin_a, fin_b))
            for (due, fa, fb) in pend:
                fb()
            pend = []
            for c in range(NB):
                by = 5 + c % 2
                self.group(by, n, lambda hh, c=c: Wo[:, hh, c * 128:(c + 1) * 128], lambda hh: oT[:, hh, 0:n], NH, [rW, roT])
                k.op(k.DVE, lambda e, c=c, by=by: e.scalar_tensor_tensor(out=xg[:, c, 0:n], in0=self.bank[by][:, 0:n], scalar=self.f_gt(0, c, jc), in1=xg[:, c, 0:n],
                                                                         op0=ALU.mult, op1=ALU.add),
                     reads=[self.rbank[by], self.rmod, rxg], writes=[rxg])
            k.dma(k.SP, self.ds("a_xo"), self.XTv[:, :, t0:t0 + n], xg[:, :, 0:n], reads=[rxg], writes=self.xt_res(t0, n))


Prog.attention = _attention


def rope_consts():
    rows = SEQ // 64
    row = np.repeat(np.arange(rows), 64).astype(np.float32)
    col = np.tile(np.arange(64), rows).astype(np.float32)
    inv = (1.0 / (np.float32(10000.0) ** (np.arange(0, 32, 2, dtype=np.float32) / np.float32(32)))).astype(np.float32)
    ang = np.stack([row[:, None] * inv, col[:, None] * inv], axis=1)
    ang = np.broadcast_to(ang[:, :, None, :], (SEQ, 2, 2, 16)).reshape(SEQ, 64)
    R = np.zeros((64, 64), np.float32)
    for m in range(64):
        if (m % 32) < 16:
            R[m + 16, m] = -1.0
        else:
            R[m - 16, m] = 1.0
    R2 = np.zeros((128, 128), np.float32)
    R2[0:64, 0:64] = R
    R2[64:128, 64:128] = R
    return {"rope_cos": np.ascontiguousarray(np.cos(ang).T.astype(np.float32)), "rope_sin": np.ascontiguousarray(np.sin(ang).T.astype(np.float32)), "rope_R": R2}


SI = 2048
SH = 32
SG = 4
SN = 128
NXC = 24
SBLK = [(0, 256, 1, 0, 256)] + [(256 + i * 256, 256, 0, 256, T) for i in range(16)]


def _ssd(self, li):
    k = self.k
    p = "l%d_" % li
    wx_d = self.inp(p + "s_wx", [128, NB, 3072 + 64], F32)
    wz_d = self.inp(p + "s_wz", [128, NB, SI], F32)
    wo_d = self.inp(p + "s_wo", [128, 16, D], F32)
    cw_d = self.inp(p + "s_cw", [128, NXC, 5], F32)
    cbf_d = self.inp(p + "s_cbf", [128, NXC], F32)
    cbr_d = self.inp(p + "s_cbr", [1, 3072], F32)
    vec_d = self.inp(p + "s_vec", [1, 5 * 32], F32)
    on_d = self.inp(p + "s_onorm", [1, SI], F32)
    tri_d = self.inp("s_tri", [128, 4, 128], F32)
    YS = self.nc.dram_tensor("YS", [T, SI], F32).ap()
    XSd = self.nc.dram_tensor("XSd", [T, SI], BF16).ap()
    BTd = self.nc.dram_tensor("BTd", [128, SG, T], BF16).ap()
    CTd = self.nc.dram_tensor("CTd", [128, SG, T], BF16).ap()
    Btd = self.nc.dram_tensor("Btd", [T, SG * 128], BF16).ap()
    rSV = {i: Res() for i in range(T // 256)}
    rYS = {i: Res() for i in range(T // 128)}
    NTl = T // 128
    for sweep in (0, 1):
        with k.phase():
            self.prep_alloc(260)
            W = k.sb([128, NB, 3072 + 64], BF16, "s_W")
            rW = Res()
            k.dma(k.POOL, self.ds("s_w"), W[:], wx_d, writes=[rW])
            cw = k.sb([128, NXC, 5], F32, "s_cw")
            cbf = k.sb([128, NXC], F32, "s_cbf")
            cbr = k.sb([1, 3072], BF16, "s_cbr")
            vec = k.sb([128, 5 * 32], F32, "s_vec")
            tri = k.sb([128, 4, 128], F32, "s_tri")
            onesf = k.sb([128, 128], F32, "s_onesf")
            rC = Res()
            k.dma(k.SP, self.ds("s_c"), cw[:], cw_d, writes=[rC])
            k.dma(k.SP, self.ds("s_c"), cbf[:], cbf_d, writes=[rC])
            k.dma(k.POOL, self.ds("s_w"), cbr[:], cbr_d, writes=[rC])
            k.dma(k.SP, self.ds("s_c"), vec[:], vec_d.partition_broadcast(128), writes=[rC])
            k.dma(k.SP, self.ds("s_c"), tri[:], tri_d, writes=[rC])
            k.op(k.DVE, lambda e: e.memset(onesf[:], 1.0), writes=[rC])
            k.op(k.ACT, lambda e: e.activation(out=vec[:, 0:64], in_=vec[:, 0:64], func=AF.Exp), reads=[rC], writes=[rC])
            k.op(k.DVE, lambda e: e.tensor_scalar(out=vec[:, 0:64], in0=vec[:, 0:64], scalar1=-1.0, scalar2=None, op0=ALU.mult), reads=[rC], writes=[rC])
            diag = k.sb([128, NXC, 5, 128], BF16, "s_diag")
            rdiag = Res()
            for ch in range(NXC):
                for tp in range(5):
                    k.op(k.DVE, lambda e, ch=ch, tp=tp: e.tensor_scalar(out=diag[:, ch, tp, :], in0=self.ident[:], scalar1=cw[:, ch, tp:tp + 1], scalar2=None, op0=ALU.mult),
                         reads=[self.rident, rC], writes=[rdiag])
            d = sweep
            TR = tri[:, d, :]
            LS = tri[:, 2 + d, :]
            xg = k.sb([128, NB, 260], F32, "s_xg")
            rxg = Res()
            hT = k.sb([128, NB, 260], BF16, "s_hT")
            rhT = Res()
            xraw = k.sb([128, NXC, 260], BF16, "s_xraw")
            rxraw = Res()
            xs = k.sb([128, 2, SI], BF16, "s_xs")
            rxs = Res()
            xdt = k.sb([128, SI], BF16, "s_xdt")
            rxdt = Res()
            xw = k.sb([128, SI], BF16, "s_xw")
            rxw = Res()
            BT = k.sb([128, SG, 256], BF16, "s_BT")
            CTt = k.sb([128, SG, 256], BF16, "s_CT")
            rBC = Res()
            Btm = k.sb([128, 2, SG, 128], BF16, "s_Btm")
            rBtm = Res()
            dts = k.sb([128, 2, 8, 32], F32, "s_dts")
            rdts = [Res(), Res()]
            adtTri = [k.sb([128, 8, 128], F32, "s_adtTri%d" % i) for i in range(2)]
            radt = [Res(), Res()]
            Dm = [k.sb([128, 8, 128], BF16, "s_Dm%d" % i) for i in range(2)]
            rDm = [Res(), Res()]
            MT = [k.sb([128, 8, 128], BF16, "s_MT%d" % i) for i in range(2)]
            rMT = [Res(), Res()]
            cbm = k.sb([128, SG, 128], F32, "s_cbm")
            rcbm = Res()
            H = k.sb([128, SI], F32, "s_H")
            Hb = k.sb([128, SI], BF16, "s_Hb")
            rH = Res()
            rHb = Res()
            k.op(k.DVE, lambda e: e.memset(H[:], 0.0), writes=[rH])
            k.op(k.DVE, lambda e: e.memset(Hb[:], 0.0), writes=[rHb])
            ysum = k.sb([128, SI], F32, "s_ysum")
            rys = Res()
            yo = [k.sb([128, 512], F32, "s_yo%d" % i) for i in range(2)]
            ryo = [Res(), Res()]
            yf = k.sb([128, SI], F32, "s_yf")
            ryf = Res()
            order = list(SBLK) if d == 0 else [SBLK[0]] + SBLK[:0:-1]
            for (t0, n, jc, slo, shi) in order:
                if d == 0:
                    lo, hi = max(t0 - 2, slo), min(t0 + n + 2, shi)
                else:
                    lo, hi = t0, t0 + n
                c0, wd = lo - (t0 - 2), hi - lo
                k.dma(k.SP, self.ds("s_xg"), xg[:, :, 0:wd], self.XTv[:, :, lo:hi], reads=self.xt_res(lo, wd), writes=[rxg])
                self.prep_h(xg, rxg, 0, wd, hT, rhT, 0, 0, jc)
                hb = (t0 - lo)
                bidx = t0 // 256
                if d == 0:
                    if c0 > 0:
                        k.op(k.DVE, lambda e: e.memset(xraw[:, :, 0:c0], 0.0), writes=[rxraw])
                    if c0 + wd < 260:
                        k.op(k.DVE, lambda e: e.memset(xraw[:, :, c0 + wd:260], 0.0), writes=[rxraw])
                    for ch in range(NXC):
                        b = ch % 2
                        self.group(b, wd, lambda kc, ch=ch: W[:, kc, ch * 128:(ch + 1) * 128], lambda kc: hT[:, kc, 0:wd], NB, [rW, rhT])
                        if ch % 2 == 0:
                            k.op(k.ACT, lambda e, b=b, ch=ch: e.copy(out=xraw[:, ch, c0:c0 + wd], in_=self.bank[b][:, 0:wd]), reads=[self.rbank[b]], writes=[rxraw])
                        else:
                            k.op(k.DVE, lambda e, b=b, ch=ch: e.tensor_copy(out=xraw[:, ch, c0:c0 + wd], in_=self.bank[b][:, 0:wd]), reads=[self.rbank[b]], writes=[rxraw])
                    for q in range(8):
                        ch = 16 + q
                        b = 2 + q % 2
                        self.group(b, n, lambda tp, ch=ch: diag[:, ch, tp, :], lambda tp, ch=ch: xraw[:, ch, tp:tp + n], 5, [rdiag, rxraw])
                        dst = BT[:, q, 0:n] if q < 4 else CTt[:, q - 4, 0:n]
                        k.op(k.ACT, lambda e, b=b, ch=ch, dst=dst: e.activation(out=dst, in_=self.bank[b][:, 0:n], func=AF.Silu, bias=cbf[:, ch:ch + 1], scale=1.0),
                             reads=[self.rbank[b], rC], writes=[rBC])
                    for s in (0, 1):
                        for cb4 in range(5):
                            b = 4 + cb4 % 2
                            for q in range(4):
                                ch = cb4 * 4 + q
                                for tp in range(5):
                                    k.op(k.PE, lambda e, b=b, q=q, ch=ch, tp=tp: e.matmul(self.bank[b][:, q * 128:(q + 1) * 128], xraw[:, ch, s * 128 + tp:s * 128 + tp + 128], diag[:, ch, tp, :],
                                                                                          start=(tp == 0), stop=False),
                                         reads=[rxraw, rdiag], writes=[self.rbank[b]], inc=False)
                                k.op(k.PE, lambda e, b=b, q=q, ch=ch: e.matmul(self.bank[b][:, q * 128:(q + 1) * 128], self.ones_bf[0:1, 0:128], cbr[0:1, ch * 128:(ch + 1) * 128],
                                                                                 start=False, stop=True),
                                     reads=[self.rones, rC], writes=[self.rbank[b]])
                            if cb4 < 4:
                                k.op(k.ACT, lambda e, b=b, cb4=cb4: e.activation(out=xs[:, s, cb4 * 512:(cb4 + 1) * 512], in_=self.bank[b][:], func=AF.Silu),
                                     reads=[self.rbank[b]], writes=[rxs])
                            else:
                                k.op(k.ACT, lambda e, b=b: e.activation(out=Btm[:, s], in_=self.bank[b][:].rearrange("p (g n) -> p g n", g=SG), func=AF.Silu),
                                     reads=[self.rbank[b]], writes=[rBtm])
                    k.dma(k.SP, self.ds("s_st0"), XSd[t0:t0 + n, :].rearrange("(s p) c -> p s c", p=128), xs[:], reads=[rxs], writes=[rSV[bidx]])
                    k.dma(k.SP, self.ds("s_st1"), BTd[:, :, t0:t0 + n], BT[:, :, 0:n], reads=[rBC], writes=[rSV[bidx]])
                    k.dma(k.SP, self.ds("s_st2"), CTd[:, :, t0:t0 + n], CTt[:, :, 0:n], reads=[rBC], writes=[rSV[bidx]])
                    k.dma(k.SP, self.ds("s_st3"), Btd[t0:t0 + n, :].rearrange("(s p) (g m) -> p s g m", p=128, g=SG), Btm[:], reads=[rBtm], writes=[rSV[bidx]])
                else:
                    k.dma(k.SP, self.ds("s_ld0"), xs[:], XSd[t0:t0 + n, :].rearrange("(s p) c -> p s c", p=128), reads=[rSV[bidx]], writes=[rxs])
                    k.dma(k.SP, self.ds("s_ld1"), BT[:, :, 0:n], BTd[:, :, t0:t0 + n], reads=[rSV[bidx]], writes=[rBC])
                    k.dma(k.SP, self.ds("s_ld2"), CTt[:, :, 0:n], CTd[:, :, t0:t0 + n], reads=[rSV[bidx]], writes=[rBC])
                    k.dma(k.SP, self.ds("s_ld3"), Btm[:], Btd[t0:t0 + n, :].rearrange("(s p) (g m) -> p s g m", p=128, g=SG), reads=[rSV[bidx]], writes=[rBtm])
                tiles = [0, 1] if d == 0 else [1, 0]
                for s in (0, 1):
                    self.group(6, 32, lambda kc: hT[:, kc, hb + s * 128:hb + (s + 1) * 128], lambda kc: W[:, kc, 3072 + d * 32:3072 + (d + 1) * 32], NB, [rW, rhT])
                    R_ = rdts[s]
                    k.op(k.DVE, lambda e: e.tensor_tensor(out=dts[:, s, 0], in0=self.bank[6][:, 0:32], in1=vec[:, 64 + d * 32:96 + d * 32], op=ALU.add), reads=[self.rbank[6], rC], writes=[R_])
                    k.op(k.ACT, lambda e: e.activation(out=dts[:, s, 0], in_=dts[:, s, 0], func=AF.Exp), reads=[R_], writes=[R_])
                    k.op(k.ACT, lambda e: e.activation(out=dts[:, s, 1], in_=dts[:, s, 0], func=AF.Ln, bias=1.0, scale=1.0), reads=[R_], writes=[R_])
                    k.op(k.DVE, lambda e: e.tensor_tensor(out=dts[:, s, 2], in0=dts[:, s, 1], in1=vec[:, d * 32:(d + 1) * 32], op=ALU.mult), reads=[R_, rC], writes=[R_])
                    k.op(k.PE, lambda e: e.matmul(self.bank[6][:, 64:96], TR, dts[:, s, 2], start=True, stop=True), reads=[R_, rC], writes=[self.rbank[6]])
                    k.op(k.PE, lambda e: e.matmul(self.bank[6][:, 128:160], onesf[:], dts[:, s, 2], start=True, stop=True), reads=[R_, rC], writes=[self.rbank[6]])
                    k.op(k.DVE, lambda e: e.tensor_copy(out=dts[:, s, 3], in_=self.bank[6][:, 64:96]), reads=[self.rbank[6]], writes=[R_])
                    k.op(k.DVE, lambda e: e.tensor_copy(out=dts[:, s, 7], in_=self.bank[6][:, 128:160]), reads=[self.rbank[6]], writes=[R_])
                    k.op(k.ACT, lambda e: e.activation(out=dts[:, s, 4], in_=dts[:, s, 7], func=AF.Exp), reads=[R_], writes=[R_])
                    k.op(k.DVE, lambda e: e.tensor_tensor(out=dts[:, s, 7], in0=dts[:, s, 7], in1=dts[:, s, 3], op=ALU.subtract), reads=[R_], writes=[R_])
                    k.op(k.ACT, lambda e: e.activation(out=dts[:, s, 5], in_=dts[:, s, 7], func=AF.Exp), reads=[R_], writes=[R_])
                    k.op(k.DVE, lambda e: e.tensor_tensor(out=dts[:, s, 5], in0=dts[:, s, 5], in1=dts[:, s, 1], op=ALU.mult), reads=[R_], writes=[R_])
                    k.op(k.ACT, lambda e: e.activation(out=dts[:, s, 6], in_=dts[:, s, 3], func=AF.Exp), reads=[R_], writes=[R_])
                for s in tiles:
                    R_ = rdts[s]
                    tile_idx = t0 // 128 + s
                    xs3 = xs[:, s, :].rearrange("p (h q) -> p h q", h=SH)
                    k.op(k.DVE, lambda e: e.tensor_tensor(out=xdt[:].rearrange("p (h q) -> p h q", h=SH), in0=xs3, in1=dts[:, s, 1].unsqueeze(2).broadcast_to([128, SH, 64]), op=ALU.mult),
                         reads=[rxs, R_], writes=[rxdt])
                    k.op(k.POOL, lambda e: e.tensor_tensor(out=xw[:].rearrange("p (h q) -> p h q", h=SH), in0=xs3, in1=dts[:, s, 5].unsqueeze(2).broadcast_to([128, SH, 64]), op=ALU.mult),
                         reads=[rxs, R_], writes=[rxw])
                    for g in range(SG):
                        k.op(k.PE, lambda e, g=g: e.matmul(self.bank[7][:, g * 128:(g + 1) * 128], BT[:, g, s * 128:(s + 1) * 128], CTt[:, g, s * 128:(s + 1) * 128], start=True, stop=True),
                             reads=[rBC], writes=[self.rbank[7]])
                    k.op(k.DVE, lambda e: e.tensor_tensor(out=cbm[:], in0=self.bank[7][:].rearrange("p (g l) -> p g l", g=SG), in1=TR.unsqueeze(1).broadcast_to([128, SG, 128]), op=ALU.mult),
                         reads=[self.rbank[7], rC], writes=[rcbm])
                    def stage_a(g):
                        i2 = g % 2
                        k.op(k.POOL, lambda e, g=g, i2=i2: e.tensor_tensor(out=adtTri[i2][:], in0=dts[:, s, 2, g * 8:(g + 1) * 8].unsqueeze(2).broadcast_to([128, 8, 128]),
                                                                          in1=TR.unsqueeze(1).broadcast_to([128, 8, 128]), op=ALU.mult),
                             reads=[R_, rC], writes=[radt[i2]])
                        for hf in range(2):
                            bd = hf + 6 * i2
                            k.op(k.PE, lambda e, hf=hf, i2=i2, bd=bd: e.matmul(self.bank[bd][:], LS, adtTri[i2][:, hf * 4:(hf + 1) * 4, :], start=True, stop=True),
                                 reads=[radt[i2], rC], writes=[self.rbank[bd]])
                            k.op(k.ACT, lambda e, hf=hf, i2=i2, bd=bd: e.activation(out=Dm[i2][:, hf * 4:(hf + 1) * 4, :], in_=self.bank[bd][:].rearrange("p (h l) -> p h l", h=4), func=AF.Exp),
                                 reads=[self.rbank[bd]], writes=[rDm[i2]])
                    def stage_b(g):
                        i2 = g % 2
                        k.op(k.POOL, lambda e, g=g, i2=i2: e.tensor_tensor(out=MT[i2][:], in0=Dm[i2][:], in1=cbm[:, g, :].unsqueeze(1).broadcast_to([128, 8, 128]), op=ALU.mult),
                             reads=[rDm[i2], rcbm], writes=[rMT[i2]])
                        by = 2 + g % 2
                        for r in range(8):
                            hh = g * 8 + r
                            k.op(k.PE, lambda e, r=r, hh=hh, by=by, i2=i2: e.matmul(self.bank[by][:, r * 64:(r + 1) * 64], MT[i2][:, r, :], xdt[:, hh * 64:(hh + 1) * 64], start=True, stop=True),
                                 reads=[rMT[i2], rxdt], writes=[self.rbank[by]])
                        bo = 4 + g % 2
                        k.op(k.PE, lambda e, g=g, bo=bo: e.matmul(self.bank[bo][:], CTt[:, g, s * 128:(s + 1) * 128], Hb[:, g * 512:(g + 1) * 512], start=True, stop=True),
                             reads=[rBC, rHb], writes=[self.rbank[bo]])
                        k.op(k.DVE, lambda e, g=g, bo=bo: e.tensor_tensor(out=yo[g % 2][:].rearrange("p (h q) -> p h q", h=8), in0=self.bank[bo][:].rearrange("p (h q) -> p h q", h=8),
                                                                          in1=dts[:, s, 6, g * 8:(g + 1) * 8].unsqueeze(2).broadcast_to([128, 8, 64]), op=ALU.mult),
                             reads=[self.rbank[bo], R_], writes=[ryo[g % 2]])
                        k.op(k.DVE, lambda e, g=g, by=by: e.tensor_tensor(out=ysum[:, g * 512:(g + 1) * 512], in0=self.bank[by][:], in1=yo[g % 2][:], op=ALU.add),
                             reads=[self.rbank[by], ryo[g % 2]], writes=[rys])
                    stage_a(0)
                    stage_a(1)
                    stage_b(0)
                    stage_a(2)
                    stage_b(1)
                    stage_a(3)
                    stage_b(2)
                    stage_b(3)
                    for g in range(SG):
                        bs_ = 6 + g % 2
                        k.op(k.PE, lambda e, g=g, bs_=bs_: e.matmul(self.bank[bs_][:], Btm[:, s, g, :], xw[:, g * 512:(g + 1) * 512], start=True, stop=True),
                             reads=[rBtm, rxw], writes=[self.rbank[bs_]])
                        Hg = H[:, g * 512:(g + 1) * 512].rearrange("p (h q) -> p h q", h=8)
                        k.op(k.POOL, lambda e, g=g, Hg=Hg: e.tensor_tensor(out=Hg, in0=Hg, in1=dts[:, s, 4, g * 8:(g + 1) * 8].unsqueeze(2).broadcast_to([128, 8, 64]), op=ALU.mult),
                             reads=[rH, R_], writes=[rH])
                        k.op(k.DVE, lambda e, g=g, bs_=bs_: e.tensor_tensor(out=H[:, g * 512:(g + 1) * 512], in0=H[:, g * 512:(g + 1) * 512], in1=self.bank[bs_][:], op=ALU.add),
                             reads=[rH, self.rbank[bs_]], writes=[rH])
                    k.op(k.ACT, lambda e: e.copy(out=Hb[:], in_=H[:]), reads=[rH], writes=[rHb])
                    if d == 1:
                        k.dma(k.SP, self.ds("s_yf"), yf[:], YS[tile_idx * 128:(tile_idx + 1) * 128, :], reads=[rYS[tile_idx]], writes=[ryf])
                        k.op(k.DVE, lambda e: e.tensor_tensor(out=ysum[:], in0=ysum[:], in1=yf[:], op=ALU.add), reads=[rys, ryf], writes=[rys])
                        k.op(k.DVE, lambda e: e.tensor_tensor(out=yf[:].rearrange("p (h q) -> p h q", h=SH), in0=xs3, in1=vec[:, 128:160].unsqueeze(2).broadcast_to([128, SH, 64]), op=ALU.mult),
                             reads=[rxs, rC, ryf], writes=[ryf])
                        k.op(k.DVE, lambda e: e.tensor_tensor(out=ysum[:], in0=ysum[:], in1=yf[:], op=ALU.add), reads=[rys, ryf], writes=[rys])
                    k.dma(k.SP, self.ds("s_yo"), YS[tile_idx * 128:(tile_idx + 1) * 128, :], ysum[:], reads=[rys], writes=[rYS[tile_idx]])
    with k.phase():
        self.prep_alloc()
        Wz = k.sb([128, NB, SI], BF16, "s_Wz")
        Wo = k.sb([128, 16, D], BF16, "s_Wo")
        onb = k.sb([128, SI], F32, "s_onb")
        identb = k.sb([128, 128], BF16, "s_identb")
        rW = Res()
        k.dma(k.POOL, self.ds("s_w"), Wz[:], wz_d, writes=[rW])
        k.dma(k.POOL, self.ds("s_w"), Wo[:], wo_d, writes=[rW])
        k.dma(k.SP, self.ds("s_c"), onb[:], on_d.partition_broadcast(128), writes=[rW])
        k.op(k.DVE, lambda e: e.tensor_copy(out=identb[:], in_=self.ident[:]), reads=[self.rident], writes=[rW])
        xg = k.sb([128, NB, 512], F32, "s3_xg")
        rxg = Res()
        hT = k.sb([128, NB, 512], BF16, "s3_hT")
        rhT = Res()
        yt = [k.sb([128, SI], F32, "s3_y%d" % i) for i in range(2)]
        ryt = [Res(), Res()]
        zs = [k.sb([128, 512], F32, "s3_zs%d" % i) for i in range(2)]
        rzs = [Res(), Res()]
        sqj = k.sb([128, SI], BF16, "s3_sqj")
        rsqj = Res()
        st = k.sb([128, 4, 4], F32, "s3_st")
        rst = [Res() for _ in range(4)]
        ynb = [k.sb([128, SI], BF16, "s3_ynb%d" % i) for i in range(2)]
        rynb = [Res(), Res()]
        ynT = k.sb([128, 16, 512], BF16, "s3_ynT")
        rynT = Res()
        pst = [k.ps([128, 1024], BF16, "s3_pst%d" % i) for i in range(0)]
        for (t0, n, jc) in BLOCKS:
            k.dma(k.SP, self.ds("s_xg"), xg[:, :, 0:n], self.XTv[:, :, t0:t0 + n], reads=self.xt_res(t0, n), writes=[rxg])
            self.prep_h(xg, rxg, 0, n, hT, rhT, 0, 0, jc)
            for s in range(n // 128):
                ti = t0 // 128 + s
                y = yt[s % 2]
                ry = ryt[s % 2]
                k.dma(k.SP, self.ds("s3_y%d" % (s % 2)), y[:], YS[ti * 128:(ti + 1) * 128, :], reads=[rYS[ti]], writes=[ry])
                for cb4 in range(4):
                    b = cb4 % 2
                    self.group(b, 512, lambda kc: hT[:, kc, s * 128:(s + 1) * 128], lambda kc, cb4=cb4: Wz[:, kc, cb4 * 512:(cb4 + 1) * 512], NB, [rW, rhT])
                    k.op(k.ACT, lambda e, b=b: e.activation(out=zs[b][:], in_=self.bank[b][:], func=AF.Silu), reads=[self.rbank[b]], writes=[rzs[b]])
                    k.op(k.DVE, lambda e, b=b, cb4=cb4: e.tensor_tensor(out=y[:, cb4 * 512:(cb4 + 1) * 512], in0=y[:, cb4 * 512:(cb4 + 1) * 512], in1=zs[b][:], op=ALU.mult),
                         reads=[ry, rzs[b]], writes=[ry])
                si = s % 4
                k.op(k.ACT, lambda e: e.activation(out=sqj[:], in_=y[:], func=AF.Square, accum_out=st[:, si, 0:1]), reads=[ry], writes=[rsqj, rst[si]])
                k.op(k.ACT, lambda e: e.activation(out=st[:, si, 1:2], in_=st[:, si, 0:1], func=AF.Ln, bias=EPS, scale=1.0 / SI), reads=[rst[si]], writes=[rst[si]])
                k.op(k.ACT, lambda e: e.activation(out=st[:, si, 2:3], in_=st[:, si, 1:2], func=AF.Exp, scale=-0.5), reads=[rst[si]], writes=[rst[si]])
                yb_ = ynb[s % 2]
                k.op(k.DVE, lambda e: e.scalar_tensor_tensor(out=yb_[:], in0=y[:], scalar=st[:, si, 2:3], in1=onb[:], op0=ALU.mult, op1=ALU.mult),
                     reads=[ry, rst[si], rW], writes=[rynb[s % 2]])
                for kc in range(16):
                    b = 2 + kc % 2
                    tp_out = self.bank[b][:].bitcast(BF16)[:, 0:128]
                    k.op(k.PE, lambda e, kc=kc, tp_out=tp_out: e.transpose(tp_out, yb_[:, kc * 128:(kc + 1) * 128], identb[:]), reads=[rynb[s % 2], rW], writes=[self.rbank[b]])
                    if kc % 2 == 0:
                        k.op(k.ACT, lambda e, kc=kc, tp_out=tp_out: e.copy(out=ynT[:, kc, s * 128:(s + 1) * 128], in_=tp_out), reads=[self.rbank[b]], writes=[rynT])
                    else:
                        k.op(k.DVE, lambda e, kc=kc, tp_out=tp_out: e.tensor_copy(out=ynT[:, kc, s * 128:(s + 1) * 128], in_=tp_out), reads=[self.rbank[b]], writes=[rynT])
            for c in range(NB):
                by = 4 + c % 2
                self.group(by, n, lambda kc, c=c: Wo[:, kc, c * 128:(c + 1) * 128], lambda kc: ynT[:, kc, 0:n], 16, [rW, rynT])
                k.op(k.DVE, lambda e, c=c, by=by: e.scalar_tensor_tensor(out=xg[:, c, 0:n], in0=self.bank[by][:, 0:n], scalar=self.f_gt(0, c, jc), in1=xg[:, c, 0:n],
                                                                         op0=ALU.mult, op1=ALU.add),
                     reads=[self.rbank[by], self.rmod, rxg], writes=[rxg])
            k.dma(k.SP, self.ds("s_xo"), self.XTv[:, :, t0:t0 + n], xg[:, :, 0:n], reads=[rxg], writes=self.xt_res(t0, n))


Prog.ssd = _ssd


def ssd_consts():
    j = np.arange(128)
    tri = np.zeros((128, 4, 128), np.float32)
    tri[:, 0, :] = (j[:, None] <= j[None, :])
    tri[:, 1, :] = (j[:, None] >= j[None, :])
    tri[:, 2, :] = (j[:, None] > j[None, :])
    tri[:, 3, :] = (j[:, None] < j[None, :])
    return {"s_tri": tri}
```

```python
import contextlib
import numpy as np
import concourse.bass as bass
import concourse.mybir as mybir
from concourse.bass_utils import run_bass_kernel_spmd

F32 = mybir.dt.float32
BF16 = mybir.dt.bfloat16
AF = mybir.ActivationFunctionType
ALU = mybir.AluOpType
AX = mybir.AxisListType

D = 1024
NB = 8
SEQ = 4096
CTX = 256
T = SEQ + CTX
DFF = 3584
NE = 8
NH = 16
NKV = 4
DH = 64
EPS = 1e-6


class Res:
    __slots__ = ("w", "r")

    def __init__(self):
        self.w = None
        self.r = {}


class Eng:
    def __init__(self, nc, es, eng, name, self_raw):
        self.eng = eng
        self.name = name
        self.sem = es.enter_context(nc.semaphore("sem_" + name))
        self.cnt = 0
        self.seen = {}
        self.self_raw = self_raw
        self.pending = False


class DSem:
    def __init__(self, nc, es, name):
        self.sem = es.enter_context(nc.semaphore("dsem_" + name))
        self.cnt = 0
        self.name = name


def _add(need, mark):
    src, val = mark
    if need.get(src, 0) < val:
        need[src] = val


class K:
    def __init__(self):
        self.nc = bass.Bass("TRN2", target_bir_lowering=False)
        nc = self.nc
        self.es = contextlib.ExitStack()
        es = self.es
        self.PE = Eng(nc, es, nc.tensor, "pe", False)
        self.ACT = Eng(nc, es, nc.scalar, "act", True)
        self.DVE = Eng(nc, es, nc.vector, "dve", True)
        self.POOL = Eng(nc, es, nc.gpsimd, "pool", True)
        self.SP = Eng(nc, es, nc.sync, "sp", False)
        self.n_inst = 0
        self._uid = 0
        self.pes = []
        self.dsems = []

    def uid(self, p):
        self._uid += 1
        return "%s%d" % (p, self._uid)

    def sb(self, shape, dt, name=None):
        es = self.pes[-1] if self.pes else self.es
        return es.enter_context(self.nc.sbuf_tensor(self.uid(name or "sb"), list(shape), dt))

    def barrier(self):
        srcs = [self.PE, self.ACT, self.DVE, self.POOL, self.SP] + self.dsems
        for E in (self.PE, self.ACT, self.DVE, self.POOL, self.SP):
            assert not E.pending
            for S in srcs:
                if S is E or S.cnt == 0:
                    continue
                if E.seen.get(S, 0) < S.cnt:
                    E.eng.wait_ge(S.sem, S.cnt)
                    E.seen[S] = S.cnt

    @contextlib.contextmanager
    def phase(self):
        self.pes.append(contextlib.ExitStack())
        try:
            yield
        finally:
            self.barrier()
            self.pes.pop().close()

    def ps(self, shape, dt, name=None):
        return self.es.enter_context(self.nc.psum_tensor(name or self.uid("ps"), list(shape), dt))

    def dsem(self, name=None):
        d = DSem(self.nc, self.es, name or self.uid("d"))
        self.dsems.append(d)
        return d

    def _waits(self, E, reads, writes):
        need = {}
        for t in reads:
            if t.w is not None:
                _add(need, t.w)
        for t in writes:
            if t.w is not None:
                _add(need, t.w)
            for s, v in t.r.items():
                _add(need, (s, v))
        for src, val in need.items():
            if src is E and not E.self_raw:
                continue
            if E.seen.get(src, 0) < val:
                E.eng.wait_ge(src.sem, val)
                E.seen[src] = val

    def op(self, E, fn, reads=(), writes=(), inc=True):
        self._waits(E, reads, writes)
        inst = fn(E.eng)
        self.n_inst += 1
        if inc:
            E.cnt += 1
            inst.then_inc(E.sem, 1)
            mark = (E, E.cnt)
            E.pending = False
        else:
            mark = (E, E.cnt + 1)
            E.pending = True
        for t in reads:
            if t.r.get(E, 0) < mark[1]:
                t.r[E] = mark[1]
        for t in writes:
            t.w = mark
            t.r = {}
        return inst

    def dma(self, Q, ds, out, in_, reads=(), writes=()):
        self._waits(Q, reads, writes)
        inst = Q.eng.dma_start(out=out, in_=in_)
        self.n_inst += 1
        ds.cnt += 16
        inst.then_inc(ds.sem, 16)
        mark = (ds, ds.cnt)
        for t in reads:
            if t.r.get(ds, 0) < mark[1]:
                t.r[ds] = mark[1]
        for t in writes:
            t.w = mark
            t.r = {}
        return inst

    def finish(self, res_list):
        self._waits(self.SP, res_list, res_list)


class Prog:
    def __init__(self, layers=(0, 1, 2, 3), skip_mixer=False, skip_ffn=False):
        self.k = K()
        k = self.k
        nc = k.nc
        self.nc = nc
        self.layers = layers
        self.skip_mixer = skip_mixer
        self.skip_ffn = skip_ffn
        self.inputs = {}
        self.x_in = self.inp("x_in", [T, D], F32)
        self.c2 = self.inp("c2", [128, NB, 2], F32)
        self.out = nc.dram_tensor("out", [SEQ, D], F32, kind="ExternalOutput").ap()
        self.XT = nc.dram_tensor("XT", [D, T], F32).ap()
        self.XTv = self.XT.rearrange("(kc p) t -> p kc t", p=128)
        self.rXT = {}
        for i in range(T // 256):
            self.rXT[i] = Res()
        self.bank = [k.ps([128, 512], F32, "bank%d" % i) for i in range(8)]
        self.rbank = [Res() for _ in range(8)]
        self.dq = {}
        self.consts()
        self.mods = {}
        for li in layers:
            self.mods[li] = (k.sb([128, 64], F32, "vec%d" % li), k.sb([128, 48, 2], F32, "mod%d" % li), k.sb([128, 2, NB, 2], F32, "gs%d" % li))

    def inp(self, name, shape, dt):
        if name in self.inputs:
            return self.inputs[name]
        ap = self.nc.dram_tensor(name, list(shape), dt, kind="ExternalInput").ap()
        self.inputs[name] = ap
        return ap

    def ds(self, name):
        if name not in self.dq:
            self.dq[name] = self.k.dsem(name)
        return self.dq[name]

    def xt_res(self, t0, n):
        return [self.rXT[i] for i in range(t0 // 256, (t0 + n + 255) // 256)]

    def consts(self):
        k = self.k
        self.ident = k.sb([128, 128], F32, "ident")
        self.rident = Res()
        k.op(k.POOL, lambda e: e.memset(self.ident[:], 0.0), writes=[self.rident])
        k.op(k.POOL, lambda e: e.affine_select(out=self.ident[:], in_=self.ident[:], pattern=[[-1, 128]],
                                                compare_op=ALU.not_equal, fill=1.0, base=0, channel_multiplier=1),
             reads=[self.rident], writes=[self.rident])
        self.ones_bf = k.sb([128, 128], BF16, "ones_bf")
        self.rones = Res()
        k.op(k.POOL, lambda e: e.memset(self.ones_bf[:], 1.0), writes=[self.rones])
        self.sel = k.sb([8, NE, 128], F32, "sel")
        self.rsel = Res()
        k.op(k.POOL, lambda e: e.memset(self.sel[:], 0.0), writes=[self.rsel])
        k.op(k.POOL, lambda e: e.affine_select(out=self.sel[:], in_=self.sel[:], pattern=[[1, NE], [0, 128]],
                                                compare_op=ALU.not_equal, fill=1.0, base=0, channel_multiplier=-1),
             reads=[self.rsel], writes=[self.rsel])
        self.c2s = k.sb([128, NB, 2], F32, "c2s")
        self.rc2 = Res()
        k.dma(k.SP, self.ds("c"), self.c2s[:], self.c2, writes=[self.rc2])
        self.sc = k.sb([128, NB, 2], BF16, "silu_c")
        self.rsc = Res()
        k.op(k.ACT, lambda e: e.activation(out=self.sc[:], in_=self.c2s[:], func=AF.Silu), reads=[self.rc2], writes=[self.rsc])

    def load_input(self):
        k = self.k
        xin = [k.sb([128, D], F32, "xin%d" % i) for i in range(2)]
        rxin = [Res(), Res()]
        xo = [k.sb([128, NB, 512], F32, "xo%d" % i) for i in range(2)]
        rxo = [Res(), Res()]
        ti = 0
        blocks = [(0, 256)] + [(256 + i * 512, 512) for i in range(8)]
        for bi, (t0, n) in enumerate(blocks):
            o = xo[bi % 2]
            ro = rxo[bi % 2]
            for s in range(n // 128):
                xi = xin[ti % 2]
                rxi = rxin[ti % 2]
                k.dma(k.SP, self.ds("xin%d" % (ti % 2)), xi[:], self.x_in[t0 + s * 128:t0 + (s + 1) * 128, :], writes=[rxi])
                for half in range(2):
                    b = 6 + half
                    for q in range(4):
                        kc = half * 4 + q
                        k.op(k.PE, lambda e, kc=kc, q=q, b=b: e.transpose(self.bank[b][:, q * 128:(q + 1) * 128], xi[:, kc * 128:(kc + 1) * 128], self.ident[:]),
                             reads=[rxi, self.rident], writes=[self.rbank[b]])
                    eng = k.ACT if half == 0 else k.DVE
                    if half == 0:
                        k.op(k.ACT, lambda e, b=b, half=half: e.copy(out=o[:, half * 4:(half + 1) * 4, s * 128:(s + 1) * 128],
                                                                      in_=self.bank[b][:].rearrange("p (q t) -> p q t", q=4)),
                             reads=[self.rbank[b]], writes=[ro])
                    else:
                        k.op(k.DVE, lambda e, b=b, half=half: e.tensor_copy(out=o[:, half * 4:(half + 1) * 4, s * 128:(s + 1) * 128],
                                                                             in_=self.bank[b][:].rearrange("p (q t) -> p q t", q=4)),
                             reads=[self.rbank[b]], writes=[ro])
                ti += 1
            k.dma(k.SP, self.ds("xo%d" % (bi % 2)), self.XTv[:, :, t0:t0 + n], o[:, :, 0:n], reads=[ro], writes=self.xt_res(t0, n))

    def store_output(self):
        k = self.k
        xi = [k.sb([128, NB, 512], F32, "so_in%d" % i) for i in range(2)]
        rxi = [Res(), Res()]
        xo = [k.sb([128, D], F32, "so_out%d" % i) for i in range(2)]
        rxo = [Res(), Res()]
        ti = 0
        for bi in range(8):
            t0 = 256 + bi * 512
            a = xi[bi % 2]
            ra = rxi[bi % 2]
            k.dma(k.SP, self.ds("so_in%d" % (bi % 2)), a[:], self.XTv[:, :, t0:t0 + 512], reads=self.xt_res(t0, 512), writes=[ra])
            for s in range(4):
                o = xo[ti % 2]
                ro = rxo[ti % 2]
                for half in range(2):
                    b = 6 + half
                    for q in range(4):
                        kc = half * 4 + q
                        k.op(k.PE, lambda e, kc=kc, q=q, b=b: e.transpose(self.bank[b][:, q * 128:(q + 1) * 128], a[:, kc, s * 128:(s + 1) * 128], self.ident[:]),
                             reads=[ra, self.rident], writes=[self.rbank[b]])
                    if half == 0:
                        k.op(k.ACT, lambda e, b=b: e.copy(out=o[:, 0:512], in_=self.bank[b][:]), reads=[self.rbank[b]], writes=[ro])
                    else:
                        k.op(k.DVE, lambda e, b=b: e.tensor_copy(out=o[:, 512:1024], in_=self.bank[b][:]), reads=[self.rbank[b]], writes=[ro])
                r0 = bi * 512 + s * 128
                k.dma(k.SP, self.ds("so_out%d" % (ti % 2)), self.out[r0:r0 + 128, :], o[:], reads=[ro])
                self.out_res.append(ro)
                ti += 1

    def modulation(self, li):
        k = self.k
        aw = self.inp("l%d_ada_w" % li, [6, 128, NB, D], F32)
        vec = self.inp("l%d_vec" % li, [128, 64], F32)
        self.aw_slot = [k.sb([128, NB, D], BF16, "aw_slot%d" % i) for i in range(2)]
        self.raw_slot = [Res(), Res()]
        vs, mod, gs = self.mods[li]
        rvs = Res()
        k.dma(k.SP, self.ds("vec"), vs[:], vec, writes=[rvs])
        rmod = Res()
        b = 7
        for m in range(6):
            sl = self.aw_slot[m % 2]
            rsl = self.raw_slot[m % 2]
            k.dma(k.POOL, self.ds("aw%d" % (m % 2)), sl[:], aw[m], writes=[rsl])
            for oc in range(8):
                col = (m * 8 + oc) * 2
                for kc in range(8):
                    k.op(k.PE, lambda e, kc=kc, oc=oc, col=col: e.matmul(self.bank[b][:, col:col + 2], sl[:, kc, oc * 128:(oc + 1) * 128],
                                                                          self.sc[:, kc, :], start=(kc == 0), stop=(kc == 7)),
                         reads=[rsl, self.rsc], writes=[self.rbank[b]], inc=(kc == 7))
        k.op(k.DVE, lambda e: e.tensor_tensor(out=mod[:], in0=self.bank[b][:, 0:96].rearrange("p (a j) -> p a j", j=2),
                                              in1=vs[:, 0:48].unsqueeze(2).broadcast_to([128, 48, 2]), op=ALU.add),
             reads=[self.rbank[b], rvs], writes=[rmod])
        for w in range(2):
            sc_ = mod[:, 8 + 24 * w:16 + 24 * w, :]
            g_ = vs[:, 48 + 8 * w:56 + 8 * w].unsqueeze(2).broadcast_to([128, NB, 2])
            k.op(k.DVE, lambda e, w=w, sc_=sc_, g_=g_: e.scalar_tensor_tensor(out=gs[:, w], in0=sc_, scalar=1.0, in1=g_, op0=ALU.add, op1=ALU.mult),
                 reads=[rmod, rvs], writes=[rmod])
        self.mod = mod
        self.gs = gs
        self.rmod = rmod
        self.f_gs = lambda w, kc, j: gs[:, w, kc, j:j + 1]
        self.f_sh = lambda w, kc, j: mod[:, 24 * w + kc, j:j + 1]
        self.f_gt = lambda w, kc, j: mod[:, 16 + 24 * w + kc, j:j + 1]

    def prep_alloc(self, w=512):
        k = self.k
        self.sq = k.sb([128, NB, w], BF16, "sq")
        self.rsq = Res()
        self.rstd = k.sb([128, w], F32, "rstd")
        self.rrstd = Res()
        self.ptmp = [k.sb([128, w], F32, "ptmp%d" % i) for i in range(2)]
        self.rptmp = [Res(), Res()]

    def prep_h(self, xg, rxg, off, n, hT, rhT, hoff, w, j):
        a, b = self.prep_h_stages(xg, rxg, off, n, hT, rhT, hoff, w, j)
        a()
        b()

    def prep_h_stages(self, xg, rxg, off, n, hT, rhT, hoff, w, j, sq=None, rsq=None):
        k = self.k
        b = 6
        sq = self.sq if sq is None else sq
        rsq = self.rsq if rsq is None else rsq

        def st_a():
            for kc in range(NB):
                k.op(k.ACT, lambda e, kc=kc: e.activation(out=sq[:, kc, 0:n], in_=xg[:, kc, off:off + n], func=AF.Square),
                     reads=[rxg], writes=[rsq])

        def st_b():
            for kc in range(NB):
                k.op(k.PE, lambda e, kc=kc: e.matmul(self.bank[b][:, 0:n], self.ones_bf[:], sq[:, kc, 0:n], start=(kc == 0), stop=(kc == NB - 1)),
                     reads=[rsq, self.rones], writes=[self.rbank[b]], inc=(kc == NB - 1))
            k.op(k.ACT, lambda e: e.activation(out=self.rstd[:, 0:n], in_=self.bank[b][:, 0:n], func=AF.Ln, bias=EPS, scale=1.0 / D),
                 reads=[self.rbank[b]], writes=[self.rrstd])
            k.op(k.ACT, lambda e: e.activation(out=self.rstd[:, 0:n], in_=self.rstd[:, 0:n], func=AF.Exp, scale=-0.5),
                 reads=[self.rrstd], writes=[self.rrstd])
            for kc in range(NB):
                tmp = self.ptmp[kc % 2]
                rtmp = self.rptmp[kc % 2]
                k.op(k.DVE, lambda e, kc=kc, tmp=tmp: e.scalar_tensor_tensor(out=tmp[:, 0:n], in0=xg[:, kc, off:off + n], scalar=self.f_gs(w, kc, j),
                                                                           in1=self.rstd[:, 0:n], op0=ALU.mult, op1=ALU.mult),
                     reads=[rxg, self.rrstd, self.rmod], writes=[rtmp])
                k.op(k.ACT, lambda e, kc=kc, tmp=tmp: e.activation(out=hT[:, kc, hoff:hoff + n], in_=tmp[:, 0:n], func=AF.Identity,
                                                                    bias=self.f_sh(w, kc, j), scale=1.0),
                     reads=[rtmp, self.rmod], writes=[rhT])
        return st_a, st_b

    def ffn_alloc(self):
        k = self.k
        self.prep_alloc()
        self.f_hT = k.sb([128, NB, 1024], BF16, "f_hT")
        self.r_hT = Res()
        self.f_xg = [k.sb([128, NB, 1024], F32, "f_xg%d" % i) for i in range(2)]
        self.r_xg = [Res(), Res()]
        self.f_aT = k.sb([128, 28, 1024], BF16, "f_aT")
        self.r_aT = [Res(), Res()]
        self.f_wgu = [k.sb([128, NB, 256], BF16, "f_wgu%d" % i) for i in range(3)]
        self.r_wgu = [Res() for _ in range(3)]
        self.f_wd = [k.sb([128, 28, 128], BF16, "f_wd%d" % i) for i in range(2)]
        self.r_wd = [Res() for _ in range(2)]
        self.f_sg = [k.sb([128, 512], BF16, "f_sg%d" % i) for i in range(2)]
        self.r_sg = [Res(), Res()]
        self.f_gate = [k.sb([128, 1024], F32, "f_gate%d" % i) for i in range(2)]
        self.r_gate = [Res(), Res()]
        self.f_gT = k.sb([8, 1024], F32, "f_gT")
        self.r_gT = Res()
        self.f_ytmp = [k.sb([128, 512], F32, "f_ytmp%d" % i) for i in range(2)]
        self.r_ytmp = [Res(), Res()]
        self.f_rw = k.sb([128, NB, NE], BF16, "f_rw")
        self.r_rw = Res()
        self.f_rb = k.sb([128, NE], F32, "f_rb")
        self.r_rb = Res()
        self.f_lg = k.sb([128, 2, 8, NE], F32, "f_lg")
        self.r_lg = [Res(), Res()]

    def ffn(self, li, moe, need_ctx):
        k = self.k
        self.ffn_alloc()
        ne = NE if moe else 1
        pre = "l%d_" % li
        wgu = self.inp(pre + "w_gu", [ne, 28, 128, NB, 256], F32)
        wd = self.inp(pre + "w_down", [ne, 8, 128, 28, 128], F32)
        if moe:
            rw = self.inp(pre + "router", [128, NB, NE], F32)
            rb = self.inp(pre + "router_b", [1, NE], F32)
            k.dma(k.POOL, self.ds("rw"), self.f_rw[:], rw, writes=[self.r_rw])
            k.dma(k.SP, self.ds("rb"), self.f_rb[:], rb.partition_broadcast(128), writes=[self.r_rb])
        groups = ([(0, 256, 1)] if need_ctx else []) + [(256 + i * 1024, 1024, 0) for i in range(4)]
        loads = []
        for gi in range(len(groups)):
            for e in range(ne):
                for jj in range(28):
                    loads.append(("gu", e, jj))
                for c in range(8):
                    loads.append(("d", e, c))
        st = {"issued": 0, "ngu": 0, "nd": 0}
        slot_of = {}

        def issue_to(idx):
            while st["issued"] <= idx and st["issued"] < len(loads):
                i = st["issued"]
                kind, e, q = loads[i]
                if kind == "gu":
                    s = st["ngu"] % 3
                    st["ngu"] += 1
                    k.dma(k.POOL, self.ds("wgu%d" % s), self.f_wgu[s][:], wgu[e, q], writes=[self.r_wgu[s]])
                else:
                    s = st["nd"] % 2
                    st["nd"] += 1
                    k.dma(k.POOL, self.ds("wd%d" % s), self.f_wd[s][:], wd[e, q], writes=[self.r_wd[s]])
                slot_of[i] = s
                st["issued"] += 1

        li_ptr = 0
        hT, rhT, aT = self.f_hT, self.r_hT, self.f_aT

        def subs_of(n_):
            return [(s_ * 512, min(512, n_ - s_ * 512)) for s_ in range((n_ + 511) // 512)]

        def load_xg(gi_):
            t0_, n_, jc_ = groups[gi_]
            k.dma(k.SP, self.ds("f_xg%d" % (gi_ % 2)), self.f_xg[gi_ % 2][:, :, 0:n_], self.XTv[:, :, t0_:t0_ + n_], reads=self.xt_res(t0_, n_), writes=[self.r_xg[gi_ % 2]])

        def pre_stages(gi_):
            t0_, n_, jc_ = groups[gi_]
            xg_, rxg_ = self.f_xg[gi_ % 2], self.r_xg[gi_ % 2]
            st_ = []
            ab = [self.prep_h_stages(xg_, rxg_, so_, sn_, hT, rhT, so_, 1, jc_) for (so_, sn_) in subs_of(n_)]
            for a_, b_ in ab:
                st_.append(a_)
                st_.append(b_)
            if moe:
                parts = self.router_parts(n_)
                prev_b = None
                for pa_, pb_ in parts:
                    def th(pa_=pa_, prev_b=prev_b):
                        pa_()
                        if prev_b is not None:
                            prev_b()
                    st_.append(th)
                    prev_b = pb_
                st_.append(prev_b)
            return st_

        load_xg(0)
        for th in pre_stages(0):
            th()
        for gi, (t0, n, jc) in enumerate(groups):
            subs = subs_of(n)
            xg, rxg = self.f_xg[gi % 2], self.r_xg[gi % 2]
            if gi + 1 < len(groups):
                load_xg(gi + 1)
                nxt = pre_stages(gi + 1)
            else:
                nxt = []
            for e in range(ne):
                if moe:
                    gsl = e % 2
                    for (so, sn) in subs:
                        k.op(k.PE, lambda e_, so=so, sn=sn: e_.matmul(self.bank[7][:, 0:sn], self.sel[:, e, :], self.f_gT[:, so:so + sn], start=True, stop=True),
                             reads=[self.rsel, self.r_gT], writes=[self.rbank[7]])
                        k.op(k.ACT, lambda e_, so=so, sn=sn: e_.copy(out=self.f_gate[gsl][:, so:so + sn], in_=self.bank[7][:, 0:sn]),
                             reads=[self.rbank[7]], writes=[self.r_gate[gsl]])
                for jj in range(28):
                    issue_to(li_ptr + 2)
                    s = slot_of[li_ptr]
                    li_ptr += 1
                    w = self.f_wgu[s]
                    rw_ = self.r_wgu[s]
                    for si, (so, sn) in enumerate(subs):
                        bg, bu = 0 + si, 2 + si
                        for kc in range(NB):
                            k.op(k.PE, lambda e_, kc=kc, bg=bg, so=so, sn=sn: e_.matmul(self.bank[bg][:, 0:sn], w[:, kc, 0:128], hT[:, kc, so:so + sn],
                                                                                        start=(kc == 0), stop=(kc == NB - 1)),
                                 reads=[rw_, rhT], writes=[self.rbank[bg]], inc=(kc == NB - 1))
                        for kc in range(NB):
                            k.op(k.PE, lambda e_, kc=kc, bu=bu, so=so, sn=sn: e_.matmul(self.bank[bu][:, 0:sn], w[:, kc, 128:256], hT[:, kc, so:so + sn],
                                                                                        start=(kc == 0), stop=(kc == NB - 1)),
                                 reads=[rw_, rhT], writes=[self.rbank[bu]], inc=(kc == NB - 1))
                        sg = self.f_sg[si]
                        k.op(k.ACT, lambda e_, bg=bg, sn=sn, sg=sg: e_.activation(out=sg[:, 0:sn], in_=self.bank[bg][:, 0:sn], func=AF.Silu),
                             reads=[self.rbank[bg]], writes=[self.r_sg[si]])
                        k.op(k.DVE, lambda e_, bu=bu, sn=sn, so=so, sg=sg, jj=jj: e_.tensor_tensor(out=aT[:, jj, so:so + sn], in0=self.bank[bu][:, 0:sn], in1=sg[:, 0:sn], op=ALU.mult),
                             reads=[self.rbank[bu], self.r_sg[si]], writes=[self.r_aT[si]])
                for c in range(8):
                    if e == ne - 1 and nxt:
                        per = (len(nxt) + 7 - c) // (8 - c)
                        for _ in range(per):
                            nxt.pop(0)()
                    issue_to(li_ptr + 1)
                    s = slot_of[li_ptr]
                    li_ptr += 1
                    w = self.f_wd[s]
                    rw_ = self.r_wd[s]
                    for si, (so, sn) in enumerate(subs):
                        by = 4 + si
                        for jj in range(28):
                            k.op(k.PE, lambda e_, jj=jj, by=by, so=so, sn=sn: e_.matmul(self.bank[by][:, 0:sn], w[:, jj, :], aT[:, jj, so:so + sn],
                                                                                        start=(jj == 0), stop=(jj == 27)),
                                 reads=[rw_, self.r_aT[si]], writes=[self.rbank[by]], inc=(jj == 27))
                        if moe:
                            yt = self.f_ytmp[si]
                            k.op(k.DVE, lambda e_, by=by, so=so, sn=sn, yt=yt: e_.tensor_tensor(out=yt[:, 0:sn], in0=self.bank[by][:, 0:sn], in1=self.f_gate[gsl][:, so:so + sn], op=ALU.mult),
                                 reads=[self.rbank[by], self.r_gate[gsl]], writes=[self.r_ytmp[si]])
                            k.op(k.DVE, lambda e_, c=c, so=so, sn=sn, yt=yt: e_.scalar_tensor_tensor(out=xg[:, c, so:so + sn], in0=yt[:, 0:sn], scalar=self.f_gt(1, c, jc),
                                                                                                    in1=xg[:, c, so:so + sn], op0=ALU.mult, op1=ALU.add),
                                 reads=[self.r_ytmp[si], self.rmod, rxg], writes=[rxg])
                        else:
                            k.op(k.DVE, lambda e_, c=c, by=by, so=so, sn=sn: e_.scalar_tensor_tensor(out=xg[:, c, so:so + sn], in0=self.bank[by][:, 0:sn], scalar=self.f_gt(1, c, jc),
                                                                                                    in1=xg[:, c, so:so + sn], op0=ALU.mult, op1=ALU.add),
                                 reads=[self.rbank[by], self.rmod, rxg], writes=[rxg])
            k.dma(k.SP, self.ds("f_xo"), self.XTv[:, :, t0:t0 + n], xg[:, :, 0:n], reads=[rxg], writes=self.xt_res(t0, n))

    def router(self, n):
        for pa, pb in self.router_parts(n):
            pa()
            pb()

    def router_parts(self, n):
        parts = []
        for ti in range(n // 128):
            parts.append(self._router_tile(ti))
        return parts

    def _router_tile(self, ti):
        k = self.k
        hT, rhT = self.f_hT, self.r_hT
        b = 7
        tsl = slice(ti * 128, (ti + 1) * 128)
        lg = self.f_lg[:, ti % 2]
        rl = self.r_lg[ti % 2]

        L, m1, e1, L2, m2, e2, dd, g_ = (lg[:, i, :] for i in range(8))

        def part_a():
            for kc in range(NB):
                k.op(k.PE, lambda e, kc=kc: e.matmul(self.bank[b][:, 0:NE], hT[:, kc, tsl], self.f_rw[:, kc, :], start=(kc == 0), stop=(kc == NB - 1)),
                     reads=[rhT, self.r_rw], writes=[self.rbank[b]], inc=(kc == NB - 1))
            k.op(k.DVE, lambda e: e.tensor_tensor(out=L, in0=self.bank[b][:, 0:NE], in1=self.f_rb[:], op=ALU.add), reads=[self.rbank[b], self.r_rb], writes=[rl])
            k.op(k.DVE, lambda e: e.reduce_max(out=m1[:, 0:1], in_=L, axis=AX.X), reads=[rl], writes=[rl])
            k.op(k.DVE, lambda e: e.tensor_scalar(out=e1, in0=L, scalar1=m1[:, 0:1], scalar2=None, op0=ALU.is_equal), reads=[rl], writes=[rl])
            k.op(k.DVE, lambda e: e.scalar_tensor_tensor(out=L2, in0=e1, scalar=-1e30, in1=L, op0=ALU.mult, op1=ALU.add), reads=[rl], writes=[rl])
            k.op(k.DVE, lambda e: e.reduce_max(out=m2[:, 0:1], in_=L2, axis=AX.X), reads=[rl], writes=[rl])
            k.op(k.DVE, lambda e: e.tensor_scalar(out=e2, in0=L2, scalar1=m2[:, 0:1], scalar2=None, op0=ALU.is_equal), reads=[rl], writes=[rl])
            k.op(k.DVE, lambda e: e.tensor_tensor(out=dd[:, 0:1], in0=m2[:, 0:1], in1=m1[:, 0:1], op=ALU.subtract), reads=[rl], writes=[rl])
            k.op(k.ACT, lambda e: e.activation(out=dd[:, 1:2], in_=dd[:, 0:1], func=AF.Sigmoid), reads=[rl], writes=[rl])
            k.op(k.DVE, lambda e: e.tensor_scalar(out=dd[:, 2:3], in0=dd[:, 1:2], scalar1=-1.0, scalar2=1.0, op0=ALU.mult, op1=ALU.add), reads=[rl], writes=[rl])
            k.op(k.DVE, lambda e: e.tensor_scalar(out=g_, in0=e1, scalar1=dd[:, 2:3], scalar2=None, op0=ALU.mult), reads=[rl], writes=[rl])
            k.op(k.DVE, lambda e: e.scalar_tensor_tensor(out=g_, in0=e2, scalar=dd[:, 1:2], in1=g_, op0=ALU.mult, op1=ALU.add), reads=[rl], writes=[rl])

        def part_b():
            k.op(k.PE, lambda e: e.transpose(self.bank[b][0:8, 128:256], g_, self.ident[:]), reads=[rl, self.rident], writes=[self.rbank[b]])
            k.op(k.ACT, lambda e: e.copy(out=self.f_gT[:, tsl], in_=self.bank[b][0:8, 128:256]), reads=[self.rbank[b]], writes=[self.r_gT])
        return part_a, part_b


def _fm(v, nchunk):
    return np.ascontiguousarray(np.asarray(v, np.float32).reshape(nchunk, 128).T)


def host_weights(inp, layers):
    w = {}
    for li in layers:
        p = "l%d_" % li
        aw = np.asarray(inp[p + "ada_w"], np.float32)
        w[p + "ada_w"] = np.ascontiguousarray(aw.reshape(NB, 128, 6, D).transpose(2, 1, 0, 3))
        vec = np.zeros((128, 64), np.float32)
        vec[:, 0:48] = _fm(inp[p + "ada_b"], 48)
        vec[:, 48:56] = _fm(inp[p + "norm_mix"], 8)
        vec[:, 56:64] = _fm(inp[p + "norm_ffn"], 8)
        w[p + "vec"] = vec
        if li % 3 == 0:
            w.update(rope_consts())
            w[p + "wqkv"] = np.ascontiguousarray(np.asarray(inp[p + "wqkv"], np.float32).reshape(NB, 128, 1536).transpose(1, 0, 2))
            w[p + "wo"] = np.ascontiguousarray(np.asarray(inp[p + "wo"], np.float32).reshape(NH, 64, D).transpose(1, 0, 2))
            w[p + "qkn"] = np.ascontiguousarray(np.tile(np.stack([np.asarray(inp[p + "q_norm"], np.float32), np.asarray(inp[p + "k_norm"], np.float32)], axis=1), (2, 1)))
        if li % 3 == 1:
            w.update(ssd_consts())
            ip = np.asarray(inp[p + "ssm_in_proj"], np.float32)
            fm3 = lambda a: np.ascontiguousarray(a.reshape(NB, 128, a.shape[1]).transpose(1, 0, 2))
            w[p + "s_wx"] = fm3(ip[:, 2048:])
            w[p + "s_wz"] = fm3(ip[:, :2048])
            w[p + "s_wo"] = np.ascontiguousarray(np.asarray(inp[p + "ssm_out_proj"], np.float32).reshape(16, 128, D).transpose(1, 0, 2))
            w[p + "s_cw"] = np.ascontiguousarray(np.asarray(inp[p + "ssm_conv_w"], np.float32).reshape(5, NXC, 128).transpose(2, 1, 0))
            w[p + "s_cbf"] = _fm(inp[p + "ssm_conv_b"], NXC)
            w[p + "s_cbr"] = np.asarray(inp[p + "ssm_conv_b"], np.float32).reshape(1, 3072)
            w[p + "s_vec"] = np.concatenate([np.asarray(inp[p + n_], np.float32) for n_ in ("ssm_a_log_fwd", "ssm_a_log_bwd", "ssm_dt_bias_fwd", "ssm_dt_bias_bwd", "ssm_d_skip")]).reshape(1, 160)
            w[p + "s_onorm"] = np.asarray(inp[p + "ssm_out_norm"], np.float32).reshape(1, SI)
        if li % 3 == 2:
            w[p + "sc_in"] = np.ascontiguousarray(np.asarray(inp[p + "sc_in_proj"], np.float32).reshape(NB, 128, 3 * D).transpose(1, 0, 2))
            w[p + "sc_out"] = np.ascontiguousarray(np.asarray(inp[p + "sc_out_proj"], np.float32).reshape(NB, 128, D).transpose(1, 0, 2))
            w[p + "sc_cw"] = np.ascontiguousarray(np.asarray(inp[p + "sc_conv_w"], np.float32).reshape(3, NB, 128).transpose(2, 1, 0))
        if li % 2 == 0:
            gu = np.asarray(inp[p + "ffn_w_gu"], np.float32)[None]
            dn = np.asarray(inp[p + "ffn_w_down"], np.float32)[None]
        else:
            gu = np.asarray(inp[p + "moe_w_gu"], np.float32)
            dn = np.asarray(inp[p + "moe_w_down"], np.float32)
            w[p + "router"] = np.ascontiguousarray(np.asarray(inp[p + "moe_router"], np.float32).reshape(NB, 128, NE).transpose(1, 0, 2))
            w[p + "router_b"] = np.asarray(inp[p + "moe_router_b"], np.float32).reshape(1, NE)
        ne = gu.shape[0]
        w[p + "w_gu"] = np.ascontiguousarray(gu.reshape(ne, NB, 128, 2, 28, 128).transpose(0, 4, 2, 1, 3, 5)).reshape(ne, 28, 128, NB, 256)
        w[p + "w_down"] = np.ascontiguousarray(dn.reshape(ne, 28, 128, 8, 128).transpose(0, 3, 2, 1, 4))
    return w


def host_core_inputs(inp, b):
    x_in = np.concatenate([np.asarray(inp["ctx"][b], np.float32), np.asarray(inp["x"][b], np.float32)], axis=0)
    c2 = np.stack([_fm(inp["c"][b], NB), _fm(inp["c_ctx"], NB)], axis=-1)
    return {"x_in": np.ascontiguousarray(x_in), "c2": np.ascontiguousarray(c2)}


def build(layers=(0, 1, 2, 3), skip_mixer=False, skip_ffn=False):
    P = Prog(layers, skip_mixer, skip_ffn)
    k = P.k
    P.out_res = []
    with k.phase():
        P.load_input()
    for li in layers:
        with k.phase():
            P.modulation(li)
        if not skip_mixer:
            with k.phase():
                P.mixer(li)
        if not skip_ffn:
            with k.phase():
                P.ffn(li, moe=(li % 2 == 1), need_ctx=(li < 3))
    with k.phase():
        P.store_output()
    for name in ("so_out0", "so_out1"):
        d = P.dq[name]
        k.SP.eng.wait_ge(d.sem, d.cnt)
    k.es.close()
    return P


def run(inp, cores=range(8), trace=False, **kw):
    P = build(**kw)
    w = host_weights(inp, P.layers)
    in_maps = []
    for b in cores:
        m = dict(w)
        m.update(host_core_inputs(inp, b))
        m = {kk: vv for kk, vv in m.items() if kk in P.inputs}
        assert set(m) == set(P.inputs), (set(P.inputs) - set(m))
        in_maps.append(m)
    if trace:
        res = run_bass_kernel_spmd(P.nc, in_maps, core_ids=list(range(len(in_maps))), trace=True)
        print("EXEC_TIME_NS", res.exec_time_ns)
    else:
        res = run_bass_kernel_spmd(P.nc, in_maps, core_ids=list(range(len(in_maps))))
    return np.stack([r["out"] for r in res.results], axis=0)


def kernel(x, c, ctx, c_ctx,
           l0_ada_w, l0_ada_b, l0_norm_mix, l0_norm_ffn, l0_wqkv, l0_q_norm, l0_k_norm, l0_wo,
           l0_ffn_w_gu, l0_ffn_w_down,
           l1_ada_w, l1_ada_b, l1_norm_mix, l1_norm_ffn, l1_ssm_in_proj, l1_ssm_conv_w, l1_ssm_conv_b,
           l1_ssm_a_log_fwd, l1_ssm_a_log_bwd, l1_ssm_dt_bias_fwd, l1_ssm_dt_bias_bwd, l1_ssm_d_skip,
           l1_ssm_out_norm, l1_ssm_out_proj, l1_moe_router, l1_moe_router_b, l1_moe_w_gu, l1_moe_w_down,
           l2_ada_w, l2_ada_b, l2_norm_mix, l2_norm_ffn, l2_sc_in_proj, l2_sc_conv_w, l2_sc_out_proj,
           l2_ffn_w_gu, l2_ffn_w_down,
           l3_ada_w, l3_ada_b, l3_norm_mix, l3_norm_ffn, l3_wqkv, l3_q_norm, l3_k_norm, l3_wo,
           l3_moe_router, l3_moe_router_b, l3_moe_w_gu, l3_moe_w_down):
    inputs = dict(
        x=x, c=c, ctx=ctx, c_ctx=c_ctx,
        l0_ada_w=l0_ada_w, l0_ada_b=l0_ada_b, l0_norm_mix=l0_norm_mix, l0_norm_ffn=l0_norm_ffn, l0_wqkv=l0_wqkv,
        l0_q_norm=l0_q_norm, l0_k_norm=l0_k_norm, l0_wo=l0_wo, l0_ffn_w_gu=l0_ffn_w_gu, l0_ffn_w_down=l0_ffn_w_down,
        l1_ada_w=l1_ada_w, l1_ada_b=l1_ada_b, l1_norm_mix=l1_norm_mix, l1_norm_ffn=l1_norm_ffn,
        l1_ssm_in_proj=l1_ssm_in_proj, l1_ssm_conv_w=l1_ssm_conv_w, l1_ssm_conv_b=l1_ssm_conv_b,
        l1_ssm_a_log_fwd=l1_ssm_a_log_fwd, l1_ssm_a_log_bwd=l1_ssm_a_log_bwd, l1_ssm_dt_bias_fwd=l1_ssm_dt_bias_fwd,
        l1_ssm_dt_bias_bwd=l1_ssm_dt_bias_bwd, l1_ssm_d_skip=l1_ssm_d_skip, l1_ssm_out_norm=l1_ssm_out_norm,
        l1_ssm_out_proj=l1_ssm_out_proj, l1_moe_router=l1_moe_router, l1_moe_router_b=l1_moe_router_b,
        l1_moe_w_gu=l1_moe_w_gu, l1_moe_w_down=l1_moe_w_down,
        l2_ada_w=l2_ada_w, l2_ada_b=l2_ada_b, l2_norm_mix=l2_norm_mix, l2_norm_ffn=l2_norm_ffn,
        l2_sc_in_proj=l2_sc_in_proj, l2_sc_conv_w=l2_sc_conv_w, l2_sc_out_proj=l2_sc_out_proj,
        l2_ffn_w_gu=l2_ffn_w_gu, l2_ffn_w_down=l2_ffn_w_down,
        l3_ada_w=l3_ada_w, l3_ada_b=l3_ada_b, l3_norm_mix=l3_norm_mix, l3_norm_ffn=l3_norm_ffn, l3_wqkv=l3_wqkv,
        l3_q_norm=l3_q_norm, l3_k_norm=l3_k_norm, l3_wo=l3_wo, l3_moe_router=l3_moe_router,
        l3_moe_router_b=l3_moe_router_b, l3_moe_w_gu=l3_moe_w_gu, l3_moe_w_down=l3_moe_w_down)
    return run(inputs).astype(np.float32)


BLOCKS = [(0, 256, 1)] + [(256 + i * 512, 512, 0) for i in range(8)]


def _mixer(self, li):
    kind = li % 3
    if kind == 0:
        self.attention(li, need_ctx=(li < 3))
    elif kind == 1:
        self.ssd(li)
    else:
        self.shortconv(li)


Prog.mixer = _mixer


def _group(self, bank, n, lhs_fn, rhs_fn, nk, reads, m0=0, m1=128):
    k = self.k
    for kc in range(nk):
        k.op(k.PE, lambda e, kc=kc: e.matmul(self.bank[bank][m0:m1, 0:n], lhs_fn(kc), rhs_fn(kc), start=(kc == 0), stop=(kc == nk - 1)),
             reads=reads, writes=[self.rbank[bank]], inc=(kc == nk - 1))


Prog.group = _group


def _shortconv(self, li):
    k = self.k
    p = "l%d_" % li
    win = self.inp(p + "sc_in", [128, NB, 3 * D], F32)
    wout = self.inp(p + "sc_out", [128, NB, D], F32)
    cw = self.inp(p + "sc_cw", [128, NB, 3], F32)
    self.prep_alloc()
    Win = k.sb([128, NB, 3 * D], BF16, "sc_Win")
    Wout = k.sb([128, NB, D], BF16, "sc_Wout")
    cws = k.sb([128, NB, 3], F32, "sc_cw")
    rW = Res()
    k.dma(k.POOL, self.ds("scw"), Win[:], win, writes=[rW])
    k.dma(k.POOL, self.ds("scw"), Wout[:], wout, writes=[rW])
    k.dma(k.SP, self.ds("scc"), cws[:], cw, writes=[rW])
    U = {1: k.sb([128, NB, CTX + 2], BF16, "sc_Uc"), 0: k.sb([128, NB, SEQ + 2], BF16, "sc_Ul")}
    rU = Res()
    for j in (0, 1):
        L = SEQ if j == 0 else CTX
        k.op(k.POOL, lambda e, j=j: e.memset(U[j][:, :, 0:1], 0.0), writes=[rU])
        k.op(k.POOL, lambda e, j=j, L=L: e.memset(U[j][:, :, L + 1:L + 2], 0.0), writes=[rU])
    xg = k.sb([128, NB, 512], F32, "sc_xg")
    rxg = Res()
    hT = k.sb([128, NB, 512], BF16, "sc_hT")
    rhT = Res()
    gT = k.sb([128, NB, 512], BF16, "sc_gT")
    rgT = Res()
    ctmp = [k.sb([128, 512], F32, "sc_ct%d" % i) for i in range(2)]
    rct = [Res(), Res()]
    vt = [k.sb([128, 512], F32, "sc_vt%d" % i) for i in range(2)]
    rvt = [Res(), Res()]
    for ps in (1, 2):
        for (t0, n, jc) in BLOCKS:
            u0 = (t0 - 256 if jc == 0 else t0) + 1
            k.dma(k.SP, self.ds("sc_xg"), xg[:, :, 0:n], self.XTv[:, :, t0:t0 + n], reads=self.xt_res(t0, n), writes=[rxg])
            self.prep_h(xg, rxg, 0, n, hT, rhT, 0, 0, jc)
            if ps == 1:
                for oc in range(NB):
                    bc, bx = oc % 2, 2 + oc % 2
                    self.group(bc, n, lambda kc, oc=oc: Win[:, kc, D + oc * 128:D + (oc + 1) * 128], lambda kc: hT[:, kc, 0:n], NB, [rW, rhT])
                    self.group(bx, n, lambda kc, oc=oc: Win[:, kc, 2 * D + oc * 128:2 * D + (oc + 1) * 128], lambda kc: hT[:, kc, 0:n], NB, [rW, rhT])
                    ct = ctmp[oc % 2]
                    k.op(k.ACT, lambda e, bc=bc, ct=ct: e.copy(out=ct[:, 0:n], in_=self.bank[bc][:, 0:n]), reads=[self.rbank[bc]], writes=[rct[oc % 2]])
                    k.op(k.DVE, lambda e, bx=bx, ct=ct, oc=oc: e.tensor_tensor(out=U[jc][:, oc, u0:u0 + n], in0=self.bank[bx][:, 0:n], in1=ct[:, 0:n], op=ALU.mult),
                         reads=[self.rbank[bx], rct[oc % 2]], writes=[rU])
            else:
                for oc in range(NB):
                    bb = oc % 2
                    self.group(bb, n, lambda kc, oc=oc: Win[:, kc, oc * 128:(oc + 1) * 128], lambda kc: hT[:, kc, 0:n], NB, [rW, rhT])
                    v = vt[oc % 2]
                    rv = rvt[oc % 2]
                    k.op(k.DVE, lambda e, oc=oc, v=v: e.tensor_scalar(out=v[:, 0:n], in0=U[jc][:, oc, u0 - 1:u0 - 1 + n], scalar1=cws[:, oc, 0:1], scalar2=None, op0=ALU.mult),
                         reads=[rU, rW], writes=[rv])
                    for tap in (1, 2):
                        k.op(k.DVE, lambda e, oc=oc, v=v, tap=tap: e.scalar_tensor_tensor(out=v[:, 0:n], in0=U[jc][:, oc, u0 - 1 + tap:u0 - 1 + tap + n], scalar=cws[:, oc, tap:tap + 1],
                                                                                 in1=v[:, 0:n], op0=ALU.mult, op1=ALU.add),
                             reads=[rU, rW, rv], writes=[rv])
                    k.op(k.DVE, lambda e, oc=oc, v=v, bb=bb: e.tensor_tensor(out=gT[:, oc, 0:n], in0=self.bank[bb][:, 0:n], in1=v[:, 0:n], op=ALU.mult),
                         reads=[self.rbank[bb], rv], writes=[rgT])
                for c in range(NB):
                    by = 4 + c % 2
                    self.group(by, n, lambda kc, c=c: Wout[:, kc, c * 128:(c + 1) * 128], lambda kc: gT[:, kc, 0:n], NB, [rW, rgT])
                    k.op(k.DVE, lambda e, c=c, by=by: e.scalar_tensor_tensor(out=xg[:, c, 0:n], in0=self.bank[by][:, 0:n], scalar=self.f_gt(0, c, jc), in1=xg[:, c, 0:n],
                                                                             op0=ALU.mult, op1=ALU.add),
                         reads=[self.rbank[by], self.rmod, rxg], writes=[rxg])
                k.dma(k.SP, self.ds("sc_xo"), self.XTv[:, :, t0:t0 + n], xg[:, :, 0:n], reads=[rxg], writes=self.xt_res(t0, n))


Prog.shortconv = _shortconv


def _attention(self, li, need_ctx):
    k = self.k
    p = "l%d_" % li
    wqkv_d = self.inp(p + "wqkv", [128, NB, 1536], F32)
    wo_d = self.inp(p + "wo", [64, NH, D], F32)
    qkn_d = self.inp(p + "qkn", [128, 2], F32)
    cos_d = self.inp("rope_cos", [64, SEQ], F32)
    sin_d = self.inp("rope_sin", [64, SEQ], F32)
    R_d = self.inp("rope_R", [128, 128], F32)
    QT_d = self.nc.dram_tensor("QT%d" % li, [64, NH, T], BF16).ap()
    rQT = {i: Res() for i in range(len(BLOCKS))}
    NT = T // 128
    KT = k.sb([128, NKV, T], BF16, "a_KT")
    rKT = Res()
    VX = k.sb([128, NT, NKV, 128], BF16, "a_VX")
    rVX = Res()
    qkn = k.sb([128, 2], F32, "a_qkn")
    onesf = k.sb([65, 64], F32, "a_onesf")
    rW = Res()
    k.dma(k.SP, self.ds("a_c"), qkn[:], qkn_d, writes=[rW])
    k.op(k.DVE, lambda e: e.memset(onesf[:], 1.0), writes=[rW])
    k.op(k.DVE, lambda e: e.memset(VX[:, :, :, 64:128], 1.0), writes=[rVX])
    k.op(k.DVE, lambda e: e.memset(KT[64:128], 0.0), writes=[rKT])
    with k.phase():
        self.prep_alloc()
        Wqkv = k.sb([128, NB, 1536], BF16, "a_Wqkv")
        Rm = k.sb([128, 128], BF16, "a_R")
        rW1 = Res()
        k.dma(k.POOL, self.ds("a_w"), Wqkv[:], wqkv_d, writes=[rW1])
        k.dma(k.POOL, self.ds("a_w"), Rm[:], R_d, writes=[rW1])
        cs = k.sb([128, 2, 512], F32, "a_cs")
        rcs = Res()
        BD = k.sb([128, 128], BF16, "a_BD")
        k.op(k.DVE, lambda e: e.memset(BD[:], 0.0), writes=[rW1])
        k.op(k.DVE, lambda e: e.memset(BD[0:64, 0:64], 1.0), writes=[rW1])
        k.op(k.DVE, lambda e: e.memset(BD[64:128, 64:128], 1.0), writes=[rW1])
        xg = k.sb([128, NB, 512], F32, "a_xg")
        rxg = Res()
        hT = k.sb([128, NB, 512], BF16, "a_hT")
        rhT = Res()
        QTo = k.sb([128, NH // 2, 512], BF16, "a_QTo")
        rQTo = Res()
        sq = [k.sb([128, 512], BF16, "a_sq%d" % i) for i in range(2)]
        rsq = [Res(), Res()]
        rstd = [k.sb([128, 512], F32, "a_rstd%d" % i) for i in range(2)]
        rrstd = [Res(), Res()]
        qn = [k.sb([128, 512], F32, "a_qn%d" % i) for i in range(2)]
        rqn = [Res(), Res()]
        qnb = [k.sb([128, 512], BF16, "a_qnb%d" % i) for i in range(2)]
        rqnb = [Res(), Res()]
        t1 = [k.sb([128, 512], F32, "a_t1%d" % i) for i in range(2)]
        rt1 = [Res(), Res()]
        t2 = [k.sb([128, 512], F32, "a_t2%d" % i) for i in range(2)]
        rt2 = [Res(), Res()]
        QT_v = QT_d.rearrange("d (pr two) t -> d pr two t", two=2)
        for bi, (t0, n, jc) in enumerate(BLOCKS):
            k.dma(k.SP, self.ds("a_xg"), xg[:, :, 0:n], self.XTv[:, :, t0:t0 + n], reads=self.xt_res(t0, n), writes=[rxg])
            if jc == 0:
                for hf in range(2):
                    k.dma(k.SP, self.ds("a_cs"), cs[hf * 64:(hf + 1) * 64, 0, 0:n], cos_d[:, t0 - 256:t0 - 256 + n], writes=[rcs])
                    k.dma(k.SP, self.ds("a_cs"), cs[hf * 64:(hf + 1) * 64, 1, 0:n], sin_d[:, t0 - 256:t0 - 256 + n], writes=[rcs])
            self.prep_h(xg, rxg, 0, n, hT, rhT, 0, 0, jc)
            do_q = (jc == 0) or need_ctx
            units = ([("q", pr) for pr in range(NH // 2)] if do_q else []) + [("k", g) for g in range(NKV)]
            for ui, (kind_, idx_) in enumerate(units):
                isq = kind_ == "q"
                M = 128 if isq else 64
                col0 = idx_ * 128 if isq else D + idx_ * 64
                i2 = ui % 2
                bq, bs, br = i2, 2 + i2, 4 + i2
                self.group(bq, n, lambda kc, col0=col0, M=M: Wqkv[:, kc, col0:col0 + M], lambda kc: hT[:, kc, 0:n], NB, [rW1, rhT], 0, M)
                k.op(k.ACT, lambda e, bq=bq, i2=i2, M=M: e.activation(out=sq[i2][0:M, 0:n], in_=self.bank[bq][0:M, 0:n], func=AF.Square),
                     reads=[self.rbank[bq]], writes=[rsq[i2]])
                k.op(k.PE, lambda e, bs=bs, i2=i2, M=M: e.matmul(self.bank[bs][0:M, 0:n], BD[0:M, 0:M], sq[i2][0:M, 0:n], start=True, stop=True),
                     reads=[rsq[i2], rW1], writes=[self.rbank[bs]])
                k.op(k.ACT, lambda e, bs=bs, i2=i2, M=M: e.activation(out=rstd[i2][0:M, 0:n], in_=self.bank[bs][0:M, 0:n], func=AF.Ln, bias=EPS, scale=1.0 / DH),
                     reads=[self.rbank[bs]], writes=[rrstd[i2]])
                k.op(k.ACT, lambda e, i2=i2, M=M: e.activation(out=rstd[i2][0:M, 0:n], in_=rstd[i2][0:M, 0:n], func=AF.Exp, scale=-0.5),
                     reads=[rrstd[i2]], writes=[rrstd[i2]])
                gain = qkn[0:M, 0:1] if isq else qkn[0:M, 1:2]
                if isq:
                    dest, rdest = QTo[:, idx_, 0:n], rQTo
                else:
                    dest, rdest = KT[0:64, idx_, t0:t0 + n], rKT
                if jc == 1:
                    k.op(k.DVE, lambda e, bq=bq, i2=i2, dest=dest, gain=gain, M=M: e.scalar_tensor_tensor(out=dest, in0=self.bank[bq][0:M, 0:n], scalar=gain, in1=rstd[i2][0:M, 0:n],
                                                                                                      op0=ALU.mult, op1=ALU.mult),
                         reads=[self.rbank[bq], rrstd[i2], rW], writes=[rdest])
                else:
                    k.op(k.DVE, lambda e, bq=bq, i2=i2, gain=gain, M=M: e.scalar_tensor_tensor(out=qn[i2][0:M, 0:n], in0=self.bank[bq][0:M, 0:n], scalar=gain, in1=rstd[i2][0:M, 0:n],
                                                                                           op0=ALU.mult, op1=ALU.mult),
                         reads=[self.rbank[bq], rrstd[i2], rW], writes=[rqn[i2]])
                    k.op(k.ACT, lambda e, i2=i2, M=M: e.copy(out=qnb[i2][0:M, 0:n], in_=qn[i2][0:M, 0:n]), reads=[rqn[i2]], writes=[rqnb[i2]])
                    k.op(k.PE, lambda e, br=br, i2=i2, M=M: e.matmul(self.bank[br][0:M, 0:n], Rm[0:M, 0:M], qnb[i2][0:M, 0:n], start=True, stop=True),
                         reads=[rqnb[i2], rW1], writes=[self.rbank[br]])
                    k.op(k.POOL, lambda e, i2=i2, M=M: e.tensor_tensor(out=t1[i2][0:M, 0:n], in0=qn[i2][0:M, 0:n], in1=cs[0:M, 0, 0:n], op=ALU.mult),
                         reads=[rqn[i2], rcs], writes=[rt1[i2]])
                    k.op(k.DVE, lambda e, br=br, i2=i2, M=M: e.tensor_tensor(out=t2[i2][0:M, 0:n], in0=self.bank[br][0:M, 0:n], in1=cs[0:M, 1, 0:n], op=ALU.mult),
                         reads=[self.rbank[br], rcs], writes=[rt2[i2]])
                    k.op(k.POOL, lambda e, i2=i2, dest=dest, M=M: e.tensor_tensor(out=dest, in0=t1[i2][0:M, 0:n], in1=t2[i2][0:M, 0:n], op=ALU.add),
                         reads=[rt1[i2], rt2[i2]], writes=[rdest])
            for s in range(n // 128):
                ti = t0 // 128 + s
                self.group(7, 256, lambda kc, s=s: hT[:, kc, s * 128:(s + 1) * 128], lambda kc: Wqkv[:, kc, D + 256:D + 512], NB, [rW1, rhT])
                k.op(k.ACT, lambda e, ti=ti: e.copy(out=VX[:, ti, :, 0:64], in_=self.bank[7][:, 0:256].rearrange("p (g d) -> p g d", g=NKV)),
                     reads=[self.rbank[7]], writes=[rVX])
            if do_q:
                for hf in range(2):
                    k.dma(k.SP, self.ds("a_qo"), QT_v[:, :, hf, t0:t0 + n], QTo[hf * 64:(hf + 1) * 64, :, 0:n], reads=[rQTo], writes=[rQT[bi]])
    with k.phase():
        Wo = k.sb([64, NH, D], BF16, "a_Wo")
        k.dma(k.POOL, self.ds("a_w"), Wo[:], wo_d, writes=[rW])
        QTi = k.sb([128, NH, 512], BF16, "a_QTi")
        rQTi = Res()
        k.op(k.DVE, lambda e: e.memset(QTi[64:128], 0.0), writes=[rQTi])
        pT = [k.sb([128, 512], BF16, "a_pT%d" % i) for i in range(4)]
        rpT = [Res() for _ in range(4)]
        oT = k.sb([64, NH, 512], BF16, "a_oT")
        roT = Res()
        ob = [k.sb([64, 512], F32, "a_ob%d" % i) for i in range(2)]
        rob = [Res(), Res()]
        rs = [k.sb([65, 512], F32, "a_rs%d" % i) for i in range(2)]
        rrs = [Res(), Res()]
        xg = k.sb([128, NB, 512], F32, "a_xg2")
        rxg = Res()
        for bi, (t0, n, jc) in enumerate(BLOCKS):
            if jc == 1 and not need_ctx:
                continue
            k.dma(k.SP, self.ds("a_qi"), QTi[0:64, :, 0:n], QT_d[:, :, t0:t0 + n], reads=[rQT[bi]], writes=[rQTi])
            k.dma(k.SP, self.ds("a_xg"), xg[:, :, 0:n], self.XTv[:, :, t0:t0 + n], reads=self.xt_res(t0, n), writes=[rxg])
            ktiles = list(range(2)) if jc == 1 else list(range(NT))
            items = [(h, idx, kt) for h in range(NH) for idx, kt in enumerate(ktiles)]
            SB = [0, 1, 7]
            LOOK = 2

            def emit_s(ii):
                h_, idx_, kt_ = items[ii]
                bs_ = SB[ii % 3]
                k.op(k.PE, lambda e: e.matmul(self.bank[bs_][:, 0:n], KT[:, h_ // 4, kt_ * 128:(kt_ + 1) * 128], QTi[:, h_, 0:n], start=True, stop=True),
                     reads=[rKT, rQTi], writes=[self.rbank[bs_]])

            for ii in range(min(LOOK, len(items))):
                emit_s(ii)
            pend = []
            for ii, (h, idx, kt) in enumerate(items):
                g = h // 4
                bo = 2 + h % 2
                bs = SB[ii % 3]
                pp = ii % 4
                if ii + LOOK < len(items):
                    emit_s(ii + LOOK)
                k.op(k.ACT, lambda e, bs=bs, pp=pp: e.activation(out=pT[pp][:, 0:n], in_=self.bank[bs][:, 0:n], func=AF.Exp, scale=DH ** -0.5),
                     reads=[self.rbank[bs]], writes=[rpT[pp]])
                k.op(k.PE, lambda e, bo=bo, kt=kt, pp=pp, idx=idx, g=g: e.matmul(self.bank[bo][0:128, 0:n], VX[:, kt, g, :], pT[pp][:, 0:n],
                                                                                  start=(idx == 0), stop=(idx == len(ktiles) - 1)),
                     reads=[rVX, rpT[pp]], writes=[self.rbank[bo]])
                for (due, fa, fb) in list(pend):
                    if ii >= due:
                        fb()
                        pend.remove((due, fa, fb))
                if idx != len(ktiles) - 1:
                    continue

                def fin_a(bo=bo, h=h):
                    k.op(k.DVE, lambda e: e.reciprocal(out=rs[h % 2][64:65, 0:n], in_=self.bank[bo][64:65, 0:n]), reads=[self.rbank[bo]], writes=[rrs[h % 2]])
                    k.op(k.ACT, lambda e: e.copy(out=ob[h % 2][:, 0:n], in_=self.bank[bo][0:64, 0:n]), reads=[self.rbank[bo]], writes=[rob[h % 2]])

                def fin_b(bo=bo, h=h):
                    k.op(k.PE, lambda e: e.matmul(self.bank[4][0:64, 0:n], onesf[64:65, 0:64], rs[h % 2][64:65, 0:n], start=True, stop=True),
                         reads=[rrs[h % 2], rW], writes=[self.rbank[4]])
                    k.op(k.DVE, lambda e: e.tensor_tensor(out=oT[:, h, 0:n], in0=ob[h % 2][:, 0:n], in1=self.bank[4][0:64, 0:n], op=ALU.mult),
                         reads=[rob[h % 2], self.rbank[4]], writes=[roT])

                fin_a()
                pend.append((ii + min(12, len(ktiles) - 2), fin_a, fin_b))
            for (due, fa, fb) in pend:
                fb()
            pend = []
            for c in range(NB):
                by = 5 + c % 2
                self.group(by, n, lambda hh, c=c: Wo[:, hh, c * 128:(c + 1) * 128], lambda hh: oT[:, hh, 0:n], NH, [rW, roT])
                k.op(k.DVE, lambda e, c=c, by=by: e.scalar_tensor_tensor(out=xg[:, c, 0:n], in0=self.bank[by][:, 0:n], scalar=self.f_gt(0, c, jc), in1=xg[:, c, 0:n],
                                                                         op0=ALU.mult, op1=ALU.add),
                     reads=[self.rbank[by], self.rmod, rxg], writes=[rxg])
            k.dma(k.SP, self.ds("a_xo"), self.XTv[:, :, t0:t0 + n], xg[:, :, 0:n], reads=[rxg], writes=self.xt_res(t0, n))


Prog.attention = _attention


def rope_consts():
    rows = SEQ // 64
    row = np.repeat(np.arange(rows), 64).astype(np.float32)
    col = np.tile(np.arange(64), rows).astype(np.float32)
    inv = (1.0 / (np.float32(10000.0) ** (np.arange(0, 32, 2, dtype=np.float32) / np.float32(32)))).astype(np.float32)
    ang = np.stack([row[:, None] * inv, col[:, None] * inv], axis=1)
    ang = np.broadcast_to(ang[:, :, None, :], (SEQ, 2, 2, 16)).reshape(SEQ, 64)
    R = np.zeros((64, 64), np.float32)
    for m in range(64):
        if (m % 32) < 16:
            R[m + 16, m] = -1.0
        else:
            R[m - 16, m] = 1.0
    R2 = np.zeros((128, 128), np.float32)
    R2[0:64, 0:64] = R
    R2[64:128, 64:128] = R
    return {"rope_cos": np.ascontiguousarray(np.cos(ang).T.astype(np.float32)), "rope_sin": np.ascontiguousarray(np.sin(ang).T.astype(np.float32)), "rope_R": R2}


SI = 2048
SH = 32
SG = 4
SN = 128
NXC = 24
SBLK = [(0, 256, 1, 0, 256)] + [(256 + i * 256, 256, 0, 256, T) for i in range(16)]


def _ssd(self, li):
    k = self.k
    p = "l%d_" % li
    wx_d = self.inp(p + "s_wx", [128, NB, 3072 + 64], F32)
    wz_d = self.inp(p + "s_wz", [128, NB, SI], F32)
    wo_d = self.inp(p + "s_wo", [128, 16, D], F32)
    cw_d = self.inp(p + "s_cw", [128, NXC, 5], F32)
    cbf_d = self.inp(p + "s_cbf", [128, NXC], F32)
    cbr_d = self.inp(p + "s_cbr", [1, 3072], F32)
    vec_d = self.inp(p + "s_vec", [1, 5 * 32], F32)
    on_d = self.inp(p + "s_onorm", [1, SI], F32)
    tri_d = self.inp("s_tri", [128, 4, 128], F32)
    YS = self.nc.dram_tensor("YS", [T, SI], F32).ap()
    XSd = self.nc.dram_tensor("XSd", [T, SI], BF16).ap()
    BTd = self.nc.dram_tensor("BTd", [128, SG, T], BF16).ap()
    CTd = self.nc.dram_tensor("CTd", [128, SG, T], BF16).ap()
    Btd = self.nc.dram_tensor("Btd", [T, SG * 128], BF16).ap()
    rSV = {i: Res() for i in range(T // 256)}
    rYS = {i: Res() for i in range(T // 128)}
    NTl = T // 128
    for sweep in (0, 1):
        with k.phase():
            self.prep_alloc(260)
            W = k.sb([128, NB, 3072 + 64], BF16, "s_W")
            rW = Res()
            k.dma(k.POOL, self.ds("s_w"), W[:], wx_d, writes=[rW])
            cw = k.sb([128, NXC, 5], F32, "s_cw")
            cbf = k.sb([128, NXC], F32, "s_cbf")
            cbr = k.sb([1, 3072], BF16, "s_cbr")
            vec = k.sb([128, 5 * 32], F32, "s_vec")
            tri = k.sb([128, 4, 128], F32, "s_tri")
            onesf = k.sb([128, 128], F32, "s_onesf")
            rC = Res()
            k.dma(k.SP, self.ds("s_c"), cw[:], cw_d, writes=[rC])
            k.dma(k.SP, self.ds("s_c"), cbf[:], cbf_d, writes=[rC])
            k.dma(k.POOL, self.ds("s_w"), cbr[:], cbr_d, writes=[rC])
            k.dma(k.SP, self.ds("s_c"), vec[:], vec_d.partition_broadcast(128), writes=[rC])
            k.dma(k.SP, self.ds("s_c"), tri[:], tri_d, writes=[rC])
            k.op(k.DVE, lambda e: e.memset(onesf[:], 1.0), writes=[rC])
            k.op(k.ACT, lambda e: e.activation(out=vec[:, 0:64], in_=vec[:, 0:64], func=AF.Exp), reads=[rC], writes=[rC])
            k.op(k.DVE, lambda e: e.tensor_scalar(out=vec[:, 0:64], in0=vec[:, 0:64], scalar1=-1.0, scalar2=None, op0=ALU.mult), reads=[rC], writes=[rC])
            diag = k.sb([128, NXC, 5, 128], BF16, "s_diag")
            rdiag = Res()
            for ch in range(NXC):
                for tp in range(5):
                    k.op(k.DVE, lambda e, ch=ch, tp=tp: e.tensor_scalar(out=diag[:, ch, tp, :], in0=self.ident[:], scalar1=cw[:, ch, tp:tp + 1], scalar2=None, op0=ALU.mult),
                         reads=[self.rident, rC], writes=[rdiag])
            d = sweep
            TR = tri[:, d, :]
            LS = tri[:, 2 + d, :]
            xg = k.sb([128, NB, 260], F32, "s_xg")
            rxg = Res()
            hT = k.sb([128, NB, 260], BF16, "s_hT")
            rhT = Res()
            xraw = k.sb([128, NXC, 260], BF16, "s_xraw")
            rxraw = Res()
            xs = k.sb([128, 2, SI], BF16, "s_xs")
            rxs = Res()
            xdt = k.sb([128, SI], BF16, "s_xdt")
            rxdt = Res()
            xw = k.sb([128, SI], BF16, "s_xw")
            rxw = Res()
            BT = k.sb([128, SG, 256], BF16, "s_BT")
            CTt = k.sb([128, SG, 256], BF16, "s_CT")
            rBC = Res()
            Btm = k.sb([128, 2, SG, 128], BF16, "s_Btm")
            rBtm = Res()
            dts = k.sb([128, 2, 8, 32], F32, "s_dts")
            rdts = [Res(), Res()]
            adtTri = [k.sb([128, 8, 128], F32, "s_adtTri%d" % i) for i in range(2)]
            radt = [Res(), Res()]
            Dm = [k.sb([128, 8, 128], BF16, "s_Dm%d" % i) for i in range(2)]
            rDm = [Res(), Res()]
            MT = [k.sb([128, 8, 128], BF16, "s_MT%d" % i) for i in range(2)]
            rMT = [Res(), Res()]
            cbm = k.sb([128, SG, 128], F32, "s_cbm")
            rcbm = Res()
            H = k.sb([128, SI], F32, "s_H")
            Hb = k.sb([128, SI], BF16, "s_Hb")
            rH = Res()
            rHb = Res()
            k.op(k.DVE, lambda e: e.memset(H[:], 0.0), writes=[rH])
            k.op(k.DVE, lambda e: e.memset(Hb[:], 0.0), writes=[rHb])
            ysum = k.sb([128, SI], F32, "s_ysum")
            rys = Res()
            yo = [k.sb([128, 512], F32, "s_yo%d" % i) for i in range(2)]
            ryo = [Res(), Res()]
            yf = k.sb([128, SI], F32, "s_yf")
            ryf = Res()
            order = list(SBLK) if d == 0 else [SBLK[0]] + SBLK[:0:-1]
            for (t0, n, jc, slo, shi) in order:
                if d == 0:
                    lo, hi = max(t0 - 2, slo), min(t0 + n + 2, shi)
                else:
                    lo, hi = t0, t0 + n
                c0, wd = lo - (t0 - 2), hi - lo
                k.dma(k.SP, self.ds("s_xg"), xg[:, :, 0:wd], self.XTv[:, :, lo:hi], reads=self.xt_res(lo, wd), writes=[rxg])
                self.prep_h(xg, rxg, 0, wd, hT, rhT, 0, 0, jc)
                hb = (t0 - lo)
                bidx = t0 // 256
                if d == 0:
                    if c0 > 0:
                        k.op(k.DVE, lambda e: e.memset(xraw[:, :, 0:c0], 0.0), writes=[rxraw])
                    if c0 + wd < 260:
                        k.op(k.DVE, lambda e: e.memset(xraw[:, :, c0 + wd:260], 0.0), writes=[rxraw])
                    for ch in range(NXC):
                        b = ch % 2
                        self.group(b, wd, lambda kc, ch=ch: W[:, kc, ch * 128:(ch + 1) * 128], lambda kc: hT[:, kc, 0:wd], NB, [rW, rhT])
                        if ch % 2 == 0:
                            k.op(k.ACT, lambda e, b=b, ch=ch: e.copy(out=xraw[:, ch, c0:c0 + wd], in_=self.bank[b][:, 0:wd]), reads=[self.rbank[b]], writes=[rxraw])
                        else:
                            k.op(k.DVE, lambda e, b=b, ch=ch: e.tensor_copy(out=xraw[:, ch, c0:c0 + wd], in_=self.bank[b][:, 0:wd]), reads=[self.rbank[b]], writes=[rxraw])
                    for q in range(8):
                        ch = 16 + q
                        b = 2 + q % 2
                        self.group(b, n, lambda tp, ch=ch: diag[:, ch, tp, :], lambda tp, ch=ch: xraw[:, ch, tp:tp + n], 5, [rdiag, rxraw])
                        dst = BT[:, q, 0:n] if q < 4 else CTt[:, q - 4, 0:n]
                        k.op(k.ACT, lambda e, b=b, ch=ch, dst=dst: e.activation(out=dst, in_=self.bank[b][:, 0:n], func=AF.Silu, bias=cbf[:, ch:ch + 1], scale=1.0),
                             reads=[self.rbank[b], rC], writes=[rBC])
                    for s in (0, 1):
                        for cb4 in range(5):
                            b = 4 + cb4 % 2
                            for q in range(4):
                                ch = cb4 * 4 + q
                                for tp in range(5):
                                    k.op(k.PE, lambda e, b=b, q=q, ch=ch, tp=tp: e.matmul(self.bank[b][:, q * 128:(q + 1) * 128], xraw[:, ch, s * 128 + tp:s * 128 + tp + 128], diag[:, ch, tp, :],
                                                                                          start=(tp == 0), stop=False),
                                         reads=[rxraw, rdiag], writes=[self.rbank[b]], inc=False)
                                k.op(k.PE, lambda e, b=b, q=q, ch=ch: e.matmul(self.bank[b][:, q * 128:(q + 1) * 128], self.ones_bf[0:1, 0:128], cbr[0:1, ch * 128:(ch + 1) * 128],
                                                                                 start=False, stop=True),
                                     reads=[self.rones, rC], writes=[self.rbank[b]])
                            if cb4 < 4:
                                k.op(k.ACT, lambda e, b=b, cb4=cb4: e.activation(out=xs[:, s, cb4 * 512:(cb4 + 1) * 512], in_=self.bank[b][:], func=AF.Silu),
                                     reads=[self.rbank[b]], writes=[rxs])
                            else:
                                k.op(k.ACT, lambda e, b=b: e.activation(out=Btm[:, s], in_=self.bank[b][:].rearrange("p (g n) -> p g n", g=SG), func=AF.Silu),
                                     reads=[self.rbank[b]], writes=[rBtm])
                    k.dma(k.SP, self.ds("s_st0"), XSd[t0:t0 + n, :].rearrange("(s p) c -> p s c", p=128), xs[:], reads=[rxs], writes=[rSV[bidx]])
                    k.dma(k.SP, self.ds("s_st1"), BTd[:, :, t0:t0 + n], BT[:, :, 0:n], reads=[rBC], writes=[rSV[bidx]])
                    k.dma(k.SP, self.ds("s_st2"), CTd[:, :, t0:t0 + n], CTt[:, :, 0:n], reads=[rBC], writes=[rSV[bidx]])
                    k.dma(k.SP, self.ds("s_st3"), Btd[t0:t0 + n, :].rearrange("(s p) (g m) -> p s g m", p=128, g=SG), Btm[:], reads=[rBtm], writes=[rSV[bidx]])
                else:
                    k.dma(k.SP, self.ds("s_ld0"), xs[:], XSd[t0:t0 + n, :].rearrange("(s p) c -> p s c", p=128), reads=[rSV[bidx]], writes=[rxs])
                    k.dma(k.SP, self.ds("s_ld1"), BT[:, :, 0:n], BTd[:, :, t0:t0 + n], reads=[rSV[bidx]], writes=[rBC])
                    k.dma(k.SP, self.ds("s_ld2"), CTt[:, :, 0:n], CTd[:, :, t0:t0 + n], reads=[rSV[bidx]], writes=[rBC])
                    k.dma(k.SP, self.ds("s_ld3"), Btm[:], Btd[t0:t0 + n, :].rearrange("(s p) (g m) -> p s g m", p=128, g=SG), reads=[rSV[bidx]], writes=[rBtm])
                tiles = [0, 1] if d == 0 else [1, 0]
                for s in (0, 1):
                    self.group(6, 32, lambda kc: hT[:, kc, hb + s * 128:hb + (s + 1) * 128], lambda kc: W[:, kc, 3072 + d * 32:3072 + (d + 1) * 32], NB, [rW, rhT])
                    R_ = rdts[s]
                    k.op(k.DVE, lambda e: e.tensor_tensor(out=dts[:, s, 0], in0=self.bank[6][:, 0:32], in1=vec[:, 64 + d * 32:96 + d * 32], op=ALU.add), reads=[self.rbank[6], rC], writes=[R_])
                    k.op(k.ACT, lambda e: e.activation(out=dts[:, s, 0], in_=dts[:, s, 0], func=AF.Exp), reads=[R_], writes=[R_])
                    k.op(k.ACT, lambda e: e.activation(out=dts[:, s, 1], in_=dts[:, s, 0], func=AF.Ln, bias=1.0, scale=1.0), reads=[R_], writes=[R_])
                    k.op(k.DVE, lambda e: e.tensor_tensor(out=dts[:, s, 2], in0=dts[:, s, 1], in1=vec[:, d * 32:(d + 1) * 32], op=ALU.mult), reads=[R_, rC], writes=[R_])
                    k.op(k.PE, lambda e: e.matmul(self.bank[6][:, 64:96], TR, dts[:, s, 2], start=True, stop=True), reads=[R_, rC], writes=[self.rbank[6]])
                    k.op(k.PE, lambda e: e.matmul(self.bank[6][:, 128:160], onesf[:], dts[:, s, 2], start=True, stop=True), reads=[R_, rC], writes=[self.rbank[6]])
                    k.op(k.DVE, lambda e: e.tensor_copy(out=dts[:, s, 3], in_=self.bank[6][:, 64:96]), reads=[self.rbank[6]], writes=[R_])
                    k.op(k.DVE, lambda e: e.tensor_copy(out=dts[:, s, 7], in_=self.bank[6][:, 128:160]), reads=[self.rbank[6]], writes=[R_])
                    k.op(k.ACT, lambda e: e.activation(out=dts[:, s, 4], in_=dts[:, s, 7], func=AF.Exp), reads=[R_], writes=[R_])
                    k.op(k.DVE, lambda e: e.tensor_tensor(out=dts[:, s, 7], in0=dts[:, s, 7], in1=dts[:, s, 3], op=ALU.subtract), reads=[R_], writes=[R_])
                    k.op(k.ACT, lambda e: e.activation(out=dts[:, s, 5], in_=dts[:, s, 7], func=AF.Exp), reads=[R_], writes=[R_])
                    k.op(k.DVE, lambda e: e.tensor_tensor(out=dts[:, s, 5], in0=dts[:, s, 5], in1=dts[:, s, 1], op=ALU.mult), reads=[R_], writes=[R_])
                    k.op(k.ACT, lambda e: e.activation(out=dts[:, s, 6], in_=dts[:, s, 3], func=AF.Exp), reads=[R_], writes=[R_])
                for s in tiles:
                    R_ = rdts[s]
                    tile_idx = t0 // 128 + s
                    xs3 = xs[:, s, :].rearrange("p (h q) -> p h q", h=SH)
                    k.op(k.DVE, lambda e: e.tensor_tensor(out=xdt[:].rearrange("p (h q) -> p h q", h=SH), in0=xs3, in1=dts[:, s, 1].unsqueeze(2).broadcast_to([128, SH, 64]), op=ALU.mult),
                         reads=[rxs, R_], writes=[rxdt])
                    k.op(k.POOL, lambda e: e.tensor_tensor(out=xw[:].rearrange("p (h q) -> p h q", h=SH), in0=xs3, in1=dts[:, s, 5].unsqueeze(2).broadcast_to([128, SH, 64]), op=ALU.mult),
                         reads=[rxs, R_], writes=[rxw])
                    for g in range(SG):
                        k.op(k.PE, lambda e, g=g: e.matmul(self.bank[7][:, g * 128:(g + 1) * 128], BT[:, g, s * 128:(s + 1) * 128], CTt[:, g, s * 128:(s + 1) * 128], start=True, stop=True),
                             reads=[rBC], writes=[self.rbank[7]])
                    k.op(k.DVE, lambda e: e.tensor_tensor(out=cbm[:], in0=self.bank[7][:].rearrange("p (g l) -> p g l", g=SG), in1=TR.unsqueeze(1).broadcast_to([128, SG, 128]), op=ALU.mult),
                         reads=[self.rbank[7], rC], writes=[rcbm])
                    def stage_a(g):
                        i2 = g % 2
                        k.op(k.POOL, lambda e, g=g, i2=i2: e.tensor_tensor(out=adtTri[i2][:], in0=dts[:, s, 2, g * 8:(g + 1) * 8].unsqueeze(2).broadcast_to([128, 8, 128]),
                                                                          in1=TR.unsqueeze(1).broadcast_to([128, 8, 128]), op=ALU.mult),
                             reads=[R_, rC], writes=[radt[i2]])
                        for hf in range(2):
                            bd = hf + 6 * i2
                            k.op(k.PE, lambda e, hf=hf, i2=i2, bd=bd: e.matmul(self.bank[bd][:], LS, adtTri[i2][:, hf * 4:(hf + 1) * 4, :], start=True, stop=True),
                                 reads=[radt[i2], rC], writes=[self.rbank[bd]])
                            k.op(k.ACT, lambda e, hf=hf, i2=i2, bd=bd: e.activation(out=Dm[i2][:, hf * 4:(hf + 1) * 4, :], in_=self.bank[bd][:].rearrange("p (h l) -> p h l", h=4), func=AF.Exp),
                                 reads=[self.rbank[bd]], writes=[rDm[i2]])
                    def stage_b(g):
                        i2 = g % 2
                        k.op(k.POOL, lambda e, g=g, i2=i2: e.tensor_tensor(out=MT[i2][:], in0=Dm[i2][:], in1=cbm[:, g, :].unsqueeze(1).broadcast_to([128, 8, 128]), op=ALU.mult),
                             reads=[rDm[i2], rcbm], writes=[rMT[i2]])
                        by = 2 + g % 2
                        for r in range(8):
                            hh = g * 8 + r
                            k.op(k.PE, lambda e, r=r, hh=hh, by=by, i2=i2: e.matmul(self.bank[by][:, r * 64:(r + 1) * 64], MT[i2][:, r, :], xdt[:, hh * 64:(hh + 1) * 64], start=True, stop=True),
                                 reads=[rMT[i2], rxdt], writes=[self.rbank[by]])
                        bo = 4 + g % 2
                        k.op(k.PE, lambda e, g=g, bo=bo: e.matmul(self.bank[bo][:], CTt[:, g, s * 128:(s + 1) * 128], Hb[:, g * 512:(g + 1) * 512], start=True, stop=True),
                             reads=[rBC, rHb], writes=[self.rbank[bo]])
                        k.op(k.DVE, lambda e, g=g, bo=bo: e.tensor_tensor(out=yo[g % 2][:].rearrange("p (h q) -> p h q", h=8), in0=self.bank[bo][:].rearrange("p (h q) -> p h q", h=8),
                                                                          in1=dts[:, s, 6, g * 8:(g + 1) * 8].unsqueeze(2).broadcast_to([128, 8, 64]), op=ALU.mult),
                             reads=[self.rbank[bo], R_], writes=[ryo[g % 2]])
                        k.op(k.DVE, lambda e, g=g, by=by: e.tensor_tensor(out=ysum[:, g * 512:(g + 1) * 512], in0=self.bank[by][:], in1=yo[g % 2][:], op=ALU.add),
                             reads=[self.rbank[by], ryo[g % 2]], writes=[rys])
                    stage_a(0)
                    stage_a(1)
                    stage_b(0)
                    stage_a(2)
                    stage_b(1)
                    stage_a(3)
                    stage_b(2)
                    stage_b(3)
                    for g in range(SG):
                        bs_ = 6 + g % 2
                        k.op(k.PE, lambda e, g=g, bs_=bs_: e.matmul(self.bank[bs_][:], Btm[:, s, g, :], xw[:, g * 512:(g + 1) * 512], start=True, stop=True),
                             reads=[rBtm, rxw], writes=[self.rbank[bs_]])
                        Hg = H[:, g * 512:(g + 1) * 512].rearrange("p (h q) -> p h q", h=8)
                        k.op(k.POOL, lambda e, g=g, Hg=Hg: e.tensor_tensor(out=Hg, in0=Hg, in1=dts[:, s, 4, g * 8:(g + 1) * 8].unsqueeze(2).broadcast_to([128, 8, 64]), op=ALU.mult),
                             reads=[rH, R_], writes=[rH])
                        k.op(k.DVE, lambda e, g=g, bs_=bs_: e.tensor_tensor(out=H[:, g * 512:(g + 1) * 512], in0=H[:, g * 512:(g + 1) * 512], in1=self.bank[bs_][:], op=ALU.add),
                             reads=[rH, self.rbank[bs_]], writes=[rH])
                    k.op(k.ACT, lambda e: e.copy(out=Hb[:], in_=H[:]), reads=[rH], writes=[rHb])
                    if d == 1:
                        k.dma(k.SP, self.ds("s_yf"), yf[:], YS[tile_idx * 128:(tile_idx + 1) * 128, :], reads=[rYS[tile_idx]], writes=[ryf])
                        k.op(k.DVE, lambda e: e.tensor_tensor(out=ysum[:], in0=ysum[:], in1=yf[:], op=ALU.add), reads=[rys, ryf], writes=[rys])
                        k.op(k.DVE, lambda e: e.tensor_tensor(out=yf[:].rearrange("p (h q) -> p h q", h=SH), in0=xs3, in1=vec[:, 128:160].unsqueeze(2).broadcast_to([128, SH, 64]), op=ALU.mult),
                             reads=[rxs, rC, ryf], writes=[ryf])
                        k.op(k.DVE, lambda e: e.tensor_tensor(out=ysum[:], in0=ysum[:], in1=yf[:], op=ALU.add), reads=[rys, ryf], writes=[rys])
                    k.dma(k.SP, self.ds("s_yo"), YS[tile_idx * 128:(tile_idx + 1) * 128, :], ysum[:], reads=[rys], writes=[rYS[tile_idx]])
    with k.phase():
        self.prep_alloc()
        Wz = k.sb([128, NB, SI], BF16, "s_Wz")
        Wo = k.sb([128, 16, D], BF16, "s_Wo")
        onb = k.sb([128, SI], F32, "s_onb")
        identb = k.sb([128, 128], BF16, "s_identb")
        rW = Res()
        k.dma(k.POOL, self.ds("s_w"), Wz[:], wz_d, writes=[rW])
        k.dma(k.POOL, self.ds("s_w"), Wo[:], wo_d, writes=[rW])
        k.dma(k.SP, self.ds("s_c"), onb[:], on_d.partition_broadcast(128), writes=[rW])
        k.op(k.DVE, lambda e: e.tensor_copy(out=identb[:], in_=self.ident[:]), reads=[self.rident], writes=[rW])
        xg = k.sb([128, NB, 512], F32, "s3_xg")
        rxg = Res()
        hT = k.sb([128, NB, 512], BF16, "s3_hT")
        rhT = Res()
        yt = [k.sb([128, SI], F32, "s3_y%d" % i) for i in range(2)]
        ryt = [Res(), Res()]
        zs = [k.sb([128, 512], F32, "s3_zs%d" % i) for i in range(2)]
        rzs = [Res(), Res()]
        sqj = k.sb([128, SI], BF16, "s3_sqj")
        rsqj = Res()
        st = k.sb([128, 4, 4], F32, "s3_st")
        rst = [Res() for _ in range(4)]
        ynb = [k.sb([128, SI], BF16, "s3_ynb%d" % i) for i in range(2)]
        rynb = [Res(), Res()]
        ynT = k.sb([128, 16, 512], BF16, "s3_ynT")
        rynT = Res()
        pst = [k.ps([128, 1024], BF16, "s3_pst%d" % i) for i in range(0)]
        for (t0, n, jc) in BLOCKS:
            k.dma(k.SP, self.ds("s_xg"), xg[:, :, 0:n], self.XTv[:, :, t0:t0 + n], reads=self.xt_res(t0, n), writes=[rxg])
            self.prep_h(xg, rxg, 0, n, hT, rhT, 0, 0, jc)
            for s in range(n // 128):
                ti = t0 // 128 + s
                y = yt[s % 2]
                ry = ryt[s % 2]
                k.dma(k.SP, self.ds("s3_y%d" % (s % 2)), y[:], YS[ti * 128:(ti + 1) * 128, :], reads=[rYS[ti]], writes=[ry])
                for cb4 in range(4):
                    b = cb4 % 2
                    self.group(b, 512, lambda kc: hT[:, kc, s * 128:(s + 1) * 128], lambda kc, cb4=cb4: Wz[:, kc, cb4 * 512:(cb4 + 1) * 512], NB, [rW, rhT])
                    k.op(k.ACT, lambda e, b=b: e.activation(out=zs[b][:], in_=self.bank[b][:], func=AF.Silu), reads=[self.rbank[b]], writes=[rzs[b]])
                    k.op(k.DVE, lambda e, b=b, cb4=cb4: e.tensor_tensor(out=y[:, cb4 * 512:(cb4 + 1) * 512], in0=y[:, cb4 * 512:(cb4 + 1) * 512], in1=zs[b][:], op=ALU.mult),
                         reads=[ry, rzs[b]], writes=[ry])
                si = s % 4
                k.op(k.ACT, lambda e: e.activation(out=sqj[:], in_=y[:], func=AF.Square, accum_out=st[:, si, 0:1]), reads=[ry], writes=[rsqj, rst[si]])
                k.op(k.ACT, lambda e: e.activation(out=st[:, si, 1:2], in_=st[:, si, 0:1], func=AF.Ln, bias=EPS, scale=1.0 / SI), reads=[rst[si]], writes=[rst[si]])
                k.op(k.ACT, lambda e: e.activation(out=st[:, si, 2:3], in_=st[:, si, 1:2], func=AF.Exp, scale=-0.5), reads=[rst[si]], writes=[rst[si]])
                yb_ = ynb[s % 2]
                k.op(k.DVE, lambda e: e.scalar_tensor_tensor(out=yb_[:], in0=y[:], scalar=st[:, si, 2:3], in1=onb[:], op0=ALU.mult, op1=ALU.mult),
                     reads=[ry, rst[si], rW], writes=[rynb[s % 2]])
                for kc in range(16):
                    b = 2 + kc % 2
                    tp_out = self.bank[b][:].bitcast(BF16)[:, 0:128]
                    k.op(k.PE, lambda e, kc=kc, tp_out=tp_out: e.transpose(tp_out, yb_[:, kc * 128:(kc + 1) * 128], identb[:]), reads=[rynb[s % 2], rW], writes=[self.rbank[b]])
                    if kc % 2 == 0:
                        k.op(k.ACT, lambda e, kc=kc, tp_out=tp_out: e.copy(out=ynT[:, kc, s * 128:(s + 1) * 128], in_=tp_out), reads=[self.rbank[b]], writes=[rynT])
                    else:
                        k.op(k.DVE, lambda e, kc=kc, tp_out=tp_out: e.tensor_copy(out=ynT[:, kc, s * 128:(s + 1) * 128], in_=tp_out), reads=[self.rbank[b]], writes=[rynT])
            for c in range(NB):
                by = 4 + c % 2
                self.group(by, n, lambda kc, c=c: Wo[:, kc, c * 128:(c + 1) * 128], lambda kc: ynT[:, kc, 0:n], 16, [rW, rynT])
                k.op(k.DVE, lambda e, c=c, by=by: e.scalar_tensor_tensor(out=xg[:, c, 0:n], in0=self.bank[by][:, 0:n], scalar=self.f_gt(0, c, jc), in1=xg[:, c, 0:n],
                                                                         op0=ALU.mult, op1=ALU.add),
                     reads=[self.rbank[by], self.rmod, rxg], writes=[rxg])
            k.dma(k.SP, self.ds("s_xo"), self.XTv[:, :, t0:t0 + n], xg[:, :, 0:n], reads=[rxg], writes=self.xt_res(t0, n))


Prog.ssd = _ssd


def ssd_consts():
    j = np.arange(128)
    tri = np.zeros((128, 4, 128), np.float32)
    tri[:, 0, :] = (j[:, None] <= j[None, :])
    tri[:, 1, :] = (j[:, None] >= j[None, :])
    tri[:, 2, :] = (j[:, None] > j[None, :])
    tri[:, 3, :] = (j[:, None] < j[None, :])
    return {"s_tri": tri}
```
